# Optimizing a Trainium2 kernel written in Bass

```python
import math
import jax
import jax.numpy as jnp
from jax import lax
import numpy as np

D_MODEL = 2048
BATCH = 4
SEQ = 2048
DEPTH = 2

MLA_HEADS = 8
MLA_Q_RANK = 768
MLA_KV_RANK = 512
MLA_NOPE = 128
MLA_ROPE = 64
MLA_QK = MLA_NOPE + MLA_ROPE
MLA_V = 128
ROPE_THETA = 10000.0

CONV_CH = 1024
CONV_WIDTH = 31

MOBA_HEADS = 8
MOBA_HEAD_DIM = 128
MOBA_BLOCK = 256
MOBA_TOPK = 3
MOBA_Q_CHUNK = 32

ATTN_Q_BLOCK = 128

D_FF = 5632

N_BRANCH = 3
NORM_EPS = 1e-6
NEG_INF = -1e30

IN_SPLITS = (MLA_Q_RANK, MLA_KV_RANK, MLA_ROPE, 2 * CONV_CH, 3 * MOBA_HEADS * MOBA_HEAD_DIM, N_BRANCH * D_MODEL)
D_IN = MLA_Q_RANK + MLA_KV_RANK + MLA_ROPE + 2 * CONV_CH + 3 * MOBA_HEADS * MOBA_HEAD_DIM + N_BRANCH * D_MODEL

kernel_name = "hybrid_mla_conformer_moba_macaron"


def rms_norm(x, g):
    xf = x.astype(jnp.float32)
    y = xf * lax.rsqrt(jnp.mean(xf * xf, axis=-1, keepdims=True) + NORM_EPS)
    return (y * g.astype(jnp.float32)).astype(x.dtype)


def layer_norm(x, g, b):
    xf = x.astype(jnp.float32)
    xc = xf - jnp.mean(xf, axis=-1, keepdims=True)
    y = xc * lax.rsqrt(jnp.mean(xc * xc, axis=-1, keepdims=True) + NORM_EPS)
    return (y * g.astype(jnp.float32) + b.astype(jnp.float32)).astype(x.dtype)


def swiglu_ffn(x, w_gate, w_up, w_down):
    return (jax.nn.silu(x @ w_gate) * (x @ w_up)) @ w_down


def rope(x, pos):
    half = x.shape[-1] // 2
    inv_freq = jnp.exp(-math.log(ROPE_THETA) * jnp.arange(half, dtype=jnp.float32) * 2.0 / x.shape[-1])
    ang = pos.astype(jnp.float32)[:, None] * inv_freq[None, :]
    cos, sin = jnp.cos(ang), jnp.sin(ang)
    xf = x.astype(jnp.float32)
    x1, x2 = xf[..., :half], xf[..., half:]
    return jnp.concatenate([x1 * cos - x2 * sin, x1 * sin + x2 * cos], axis=-1).astype(x.dtype)


def alibi_slopes(n_heads):
    return jnp.exp2(-8.0 * jnp.arange(1, n_heads + 1, dtype=jnp.float32) / n_heads)


def causal_attention(q, k, v, scale):
    B, H, S, Dqk = q.shape
    nq = S // ATTN_Q_BLOCK
    qb = jnp.moveaxis(q.reshape(B, H, nq, ATTN_Q_BLOCK, Dqk), 2, 0)
    kpos = jnp.arange(S, dtype=jnp.int32)

    def one_block(args):
        qi, i = args
        qpos = i * ATTN_Q_BLOCK + jnp.arange(ATTN_Q_BLOCK, dtype=jnp.int32)
        s = jnp.einsum('bhqd,bhkd->bhqk', qi, k, preferred_element_type=jnp.float32) * scale
        s = jnp.where(kpos[None, :] <= qpos[:, None], s, NEG_INF)
        p = jax.nn.softmax(s, axis=-1)
        return jnp.einsum('bhqk,bhkd->bhqd', p.astype(v.dtype), v)

    out = lax.map(one_block, (qb, jnp.arange(nq, dtype=jnp.int32)))
    return jnp.moveaxis(out, 0, 2).reshape(B, H, S, v.shape[-1])


def moba_attention(q, k, v, slopes):
    B, H, S, Dh = q.shape
    L = MOBA_BLOCK
    nb = -(-S // L)
    pad = nb * L - S
    k_blk = jnp.pad(k, ((0, 0), (0, 0), (0, pad), (0, 0))).reshape(B, H, nb, L, Dh)
    v_blk = jnp.pad(v, ((0, 0), (0, 0), (0, pad), (0, 0))).reshape(B, H, nb, L, Dh)
    pos = jnp.arange(S, dtype=jnp.int32)
    own = pos // L
    own_idx = jnp.broadcast_to(own[None, None, :, None], (B, H, S, 1))
    n_sel = min(MOBA_TOPK, nb - 1)
    if n_sel > 0:
        k_mean = jnp.mean(k_blk.astype(jnp.float32), axis=3)
        gate = jnp.einsum('bhsd,bhnd->bhsn', q.astype(jnp.float32), k_mean)
        fully_past = jnp.arange(nb, dtype=jnp.int32)[None, :] < own[:, None]
        gate = jnp.where(fully_past, gate, NEG_INF)
        _, top_idx = lax.top_k(gate, n_sel)
        top_idx = top_idx.astype(jnp.int32)
        idx = jnp.concatenate([top_idx, own_idx], axis=-1)
        valid = jnp.concatenate([top_idx < own[:, None], jnp.ones_like(own_idx, dtype=bool)], axis=-1)
    else:
        idx = own_idx
        valid = jnp.ones_like(own_idx, dtype=bool)
    J = idx.shape[-1]
    C = MOBA_Q_CHUNK
    nc = S // C
    scale = Dh ** -0.5
    bi = jnp.arange(B)[:, None, None, None]
    hi = jnp.arange(H)[None, :, None, None]

    def to_chunks(a):
        return jnp.moveaxis(a.reshape(B, H, nc, C, a.shape[-1]), 2, 0)

    def one_chunk(args):
        qc, idc, vc, qpos = args
        k_sel = k_blk[bi, hi, idc]
        v_sel = v_blk[bi, hi, idc]
        kpos = idc[..., None] * L + jnp.arange(L, dtype=jnp.int32)
        dist = qpos[None, None, :, None, None] - kpos
        s = jnp.einsum('bhcd,bhcjld->bhcjl', qc, k_sel, preferred_element_type=jnp.float32) * scale
        s = s - slopes[None, :, None, None, None] * dist.astype(jnp.float32)
        s = jnp.where(vc[..., None] & (dist >= 0), s, NEG_INF)
        p = jax.nn.softmax(s.reshape(B, H, C, J * L), axis=-1).reshape(B, H, C, J, L)
        return jnp.einsum('bhcjl,bhcjld->bhcd', p.astype(v.dtype), v_sel)

    out = lax.map(one_chunk, (to_chunks(q), to_chunks(idx), to_chunks(valid), pos.reshape(nc, C)))
    return jnp.moveaxis(out, 0, 2).reshape(B, H, S, Dh)


def mla_branch(c_q, c_kv, k_r, g_cq, g_ckv, w_uq, w_ukv, g_qn, g_kn, w_o):
    B, S, _ = c_q.shape
    pos = jnp.arange(S, dtype=jnp.int32)
    q = (rms_norm(c_q, g_cq) @ w_uq).reshape(B, S, MLA_HEADS, MLA_QK).transpose(0, 2, 1, 3)
    kv = (rms_norm(c_kv, g_ckv) @ w_ukv).reshape(B, S, MLA_HEADS, MLA_NOPE + MLA_V).transpose(0, 2, 1, 3)
    k_nope, v = kv[..., :MLA_NOPE], kv[..., MLA_NOPE:]
    k_rot = jnp.broadcast_to(k_r[:, None], (B, MLA_HEADS, S, MLA_ROPE))
    k = jnp.concatenate([k_nope, k_rot], axis=-1)
    q = rms_norm(q, g_qn)
    k = rms_norm(k, g_kn)
    q = jnp.concatenate([q[..., :MLA_NOPE], rope(q[..., MLA_NOPE:], pos)], axis=-1)
    k = jnp.concatenate([k[..., :MLA_NOPE], rope(k[..., MLA_NOPE:], pos)], axis=-1)
    o = causal_attention(q, k, v, MLA_QK ** -0.5)
    return o.transpose(0, 2, 1, 3).reshape(B, S, MLA_HEADS * MLA_V) @ w_o


def conv_branch(u, w_dw, b_dw, g_ln, b_ln, w_pw):
    a, gate = jnp.split(u, 2, axis=-1)
    z = a * jax.nn.sigmoid(gate)
    z = lax.conv_general_dilated(z, w_dw[:, None, :], window_strides=(1,),
                                 padding=[(CONV_WIDTH - 1, 0)],
                                 dimension_numbers=('NWC', 'WIO', 'NWC'),
                                 feature_group_count=CONV_CH) + b_dw
    z = jax.nn.silu(layer_norm(z, g_ln, b_ln))
    return z @ w_pw


def moba_branch(qkv, g_qn, g_kn, w_o):
    B, S, _ = qkv.shape
    q, k, v = [t.reshape(B, S, MOBA_HEADS, MOBA_HEAD_DIM).transpose(0, 2, 1, 3) for t in jnp.split(qkv, 3, axis=-1)]
    q = rms_norm(q, g_qn)
    k = rms_norm(k, g_kn)
    o = moba_attention(q, k, v, alibi_slopes(MOBA_HEADS))
    return o.transpose(0, 2, 1, 3).reshape(B, S, MOBA_HEADS * MOBA_HEAD_DIM) @ w_o


def mixer_layer(h, w_in, b_gate, mla_cq_norm, mla_ckv_norm, mla_w_uq, mla_w_ukv, mla_q_norm, mla_k_norm, mla_w_o,
                conv_w_dw, conv_b_dw, conv_ln_g, conv_ln_b, conv_w_pw, moba_q_norm, moba_k_norm, moba_w_o, w_out):
    B, S, D = h.shape
    u = h @ w_in
    split_at = [int(i) for i in np.cumsum(IN_SPLITS)[:-1]]
    c_q, c_kv, k_r, conv_in, moba_qkv, gate_logits = jnp.split(u, split_at, axis=-1)
    y_mla = mla_branch(c_q, c_kv, k_r, mla_cq_norm, mla_ckv_norm, mla_w_uq, mla_w_ukv, mla_q_norm, mla_k_norm, mla_w_o)
    y_conv = conv_branch(conv_in, conv_w_dw, conv_b_dw, conv_ln_g, conv_ln_b, conv_w_pw)
    y_moba = moba_branch(moba_qkv, moba_q_norm, moba_k_norm, moba_w_o)
    g = jax.nn.sigmoid(gate_logits + b_gate).reshape(B, S, N_BRANCH, D)
    merged = g[:, :, 0] * y_mla + g[:, :, 1] * y_conv + g[:, :, 2] * y_moba
    return merged @ w_out


def setup_inputs(seed: int = 0) -> dict:
    key = jax.random.key(seed)
    ks = iter(jax.random.split(key, 40))

    def nrm(shape, scale):
        return scale * jax.random.normal(next(ks), shape, jnp.float32)

    def gain(shape):
        return 1.0 + 0.02 * jax.random.normal(next(ks), shape, jnp.float32)

    L, D = DEPTH, D_MODEL
    return {
        'x': nrm((BATCH, SEQ, D), 1.0),
        'ffn1_norm': gain((L, D)),
        'ffn1_w_gate': nrm((L, D, D_FF), D ** -0.5),
        'ffn1_w_up': nrm((L, D, D_FF), D ** -0.5),
        'ffn1_w_down': nrm((L, D_FF, D), D_FF ** -0.5),
        'mix_norm': gain((L, D)),
        'w_in': nrm((L, D, D_IN), D ** -0.5),
        'b_gate': nrm((L, N_BRANCH * D), 0.02),
        'mla_cq_norm': gain((L, MLA_Q_RANK)),
        'mla_ckv_norm': gain((L, MLA_KV_RANK)),
        'mla_w_uq': nrm((L, MLA_Q_RANK, MLA_HEADS * MLA_QK), MLA_Q_RANK ** -0.5),
        'mla_w_ukv': nrm((L, MLA_KV_RANK, MLA_HEADS * (MLA_NOPE + MLA_V)), MLA_KV_RANK ** -0.5),
        'mla_q_norm': gain((L, MLA_QK)),
        'mla_k_norm': gain((L, MLA_QK)),
        'mla_w_o': nrm((L, MLA_HEADS * MLA_V, D), (MLA_HEADS * MLA_V) ** -0.5),
        'conv_w_dw': nrm((L, CONV_WIDTH, CONV_CH), CONV_WIDTH ** -0.5),
        'conv_b_dw': nrm((L, CONV_CH), 0.02),
        'conv_ln_g': gain((L, CONV_CH)),
        'conv_ln_b': nrm((L, CONV_CH), 0.02),
        'conv_w_pw': nrm((L, CONV_CH, D), CONV_CH ** -0.5),
        'moba_q_norm': gain((L, MOBA_HEAD_DIM)),
        'moba_k_norm': gain((L, MOBA_HEAD_DIM)),
        'moba_w_o': nrm((L, MOBA_HEADS * MOBA_HEAD_DIM, D), (MOBA_HEADS * MOBA_HEAD_DIM) ** -0.5),
        'w_out': nrm((L, D, D), D ** -0.5),
        'ffn2_norm': gain((L, D)),
        'ffn2_w_gate': nrm((L, D, D_FF), D ** -0.5),
        'ffn2_w_up': nrm((L, D, D_FF), D ** -0.5),
        'ffn2_w_down': nrm((L, D_FF, D), D_FF ** -0.5),
    }


def reference(x, ffn1_norm, ffn1_w_gate, ffn1_w_up, ffn1_w_down, mix_norm, w_in, b_gate,
              mla_cq_norm, mla_ckv_norm, mla_w_uq, mla_w_ukv, mla_q_norm, mla_k_norm, mla_w_o,
              conv_w_dw, conv_b_dw, conv_ln_g, conv_ln_b, conv_w_pw,
              moba_q_norm, moba_k_norm, moba_w_o, w_out,
              ffn2_norm, ffn2_w_gate, ffn2_w_up, ffn2_w_down):
    for l in range(DEPTH):
        x = x + 0.5 * swiglu_ffn(rms_norm(x, ffn1_norm[l]), ffn1_w_gate[l], ffn1_w_up[l], ffn1_w_down[l])
        x = x + mixer_layer(rms_norm(x, mix_norm[l]), w_in[l], b_gate[l],
                            mla_cq_norm[l], mla_ckv_norm[l], mla_w_uq[l], mla_w_ukv[l],
                            mla_q_norm[l], mla_k_norm[l], mla_w_o[l],
                            conv_w_dw[l], conv_b_dw[l], conv_ln_g[l], conv_ln_b[l], conv_w_pw[l],
                            moba_q_norm[l], moba_k_norm[l], moba_w_o[l], w_out[l])
        x = x + 0.5 * swiglu_ffn(rms_norm(x, ffn2_norm[l]), ffn2_w_gate[l], ffn2_w_up[l], ffn2_w_down[l])
    return x
```

```python
import numpy as np
from contextlib import ExitStack
import concourse.bass as bass
import concourse.mybir as mybir
from concourse.bass_utils import run_bass_kernel_spmd

F32 = mybir.dt.float32
BF16 = mybir.dt.bfloat16
AF = mybir.ActivationFunctionType
ALU = mybir.AluOpType
AX = mybir.AxisListType

D = 2048
SEQ = 2048
NB = 4
DEPTH = 2
DFF = 5632
NCH = D // 128
T = 1024
TT = 512
NT = T // TT
FCH = DFF // 128
EPS = 1e-6

SAME_ENGINE_SYNC = True


class Buf:
    __slots__ = ("name", "writer", "readers", "excl")

    def __init__(self, name):
        self.name = name
        self.writer = None
        self.readers = []
        self.excl = False


class TV:
    __slots__ = ("ap", "bufs", "tile")

    def __init__(self, ap, bufs, tile=None):
        self.ap = ap
        self.bufs = bufs
        self.tile = tile


class Tile:
    def __init__(self, k, name, shape, dtype, space="sbuf"):
        self.k = k
        self.name = name
        self.shape = shape
        if space == "sbuf":
            self.t = k.es.enter_context(k.nc.sbuf_tensor(name, shape, dtype))
        else:
            self.t = k.es.enter_context(k.nc.psum_tensor(name, shape, dtype))
        self.buf = Buf(name)
        self.buf.excl = (space != "sbuf")
        self.dsem = None
        self.dcount = 0

    def __getitem__(self, idx):
        return TV(self.t[idx], [self.buf], self)

    def view(self, fn):
        return TV(fn(self.t), [self.buf], self)


class DTile:
    def __init__(self, k, name, shape, dtype, kind):
        self.k = k
        self.name = name
        self.h = k.nc.dram_tensor(name, shape, dtype, kind=kind)
        self.ap = self.h.ap()
        self.bufs = {}

    def v(self, ap, *keys):
        bl = []
        for key in keys:
            if key not in self.bufs:
                self.bufs[key] = Buf(f"{self.name}:{key}")
            bl.append(self.bufs[key])
        return TV(ap, bl, None)


class Eng:
    def __init__(self, k, name, eng):
        self.name = name
        self.eng = eng
        self.sem = k.es.enter_context(k.nc.semaphore("s_" + name))
        self.n = 0
        self.seen = {}


class KB:
    def __init__(self, nc, es):
        self.nc = nc
        self.es = es
        self.PE = Eng(self, "pe", nc.tensor)
        self.ACT = Eng(self, "act", nc.scalar)
        self.DVE = Eng(self, "dve", nc.vector)
        self.POOL = Eng(self, "pool", nc.gpsimd)
        self.SP = Eng(self, "sp", nc.sync)
        self.sems = {}
        for e in (self.PE, self.ACT, self.DVE, self.POOL, self.SP):
            self.sems[id(e.sem)] = e.sem
        self.planning = False
        self.nsem = 5
        self.psum = [Tile(self, f"ps{i}", [128, 512], F32, "psum") for i in range(8)]
        self.psi = 0
        self.rr = 8
        self.stats = {"waits": 0, "ops": 0, "dmas": 0}

    def new_dsem(self, name):
        s = self.es.enter_context(self.nc.semaphore("d_" + name))
        self.sems[id(s)] = s
        self.nsem += 1
        return [s, 0]

    def set_rr(self, n):
        self.rr = n
        self.psi = 0

    def ps(self):
        p = self.psum[self.psi]
        self.psi = (self.psi + 1) % self.rr
        return p

    def _sync(self, E, reads, writes):
        deps = {}
        for tv in reads:
            for b in tv.bufs:
                if b.writer is not None:
                    s, v = b.writer
                    deps[s] = max(deps.get(s, 0), v)
                if b.excl:
                    for (s, v) in b.readers:
                        if s != id(E.sem):
                            deps[s] = max(deps.get(s, 0), v)
        for tv in writes:
            for b in tv.bufs:
                if b.writer is not None:
                    s, v = b.writer
                    deps[s] = max(deps.get(s, 0), v)
                for (s, v) in b.readers:
                    deps[s] = max(deps.get(s, 0), v)
        for s, v in deps.items():
            if s == id(E.sem) and (E is self.PE or not SAME_ENGINE_SYNC):
                continue
            if E.seen.get(s, 0) < v:
                E.eng.wait_ge(self.sems[s], v)
                E.seen[s] = v
                self.stats["waits"] += 1

    def _mark(self, key, val, reads, writes):
        for tv in reads:
            for b in tv.bufs:
                b.readers.append((key, val))
        for tv in writes:
            for b in tv.bufs:
                b.writer = (key, val)
                b.readers = []

    def op(self, E, fn, reads, writes):
        if self.planning:
            return
        self._sync(E, reads, writes)
        ins = fn()
        E.n += 1
        ins.then_inc(E.sem, 1)
        self._mark(id(E.sem), E.n, reads, writes)
        self.stats["ops"] += 1

    def dma(self, Q, out, in_, dsem):
        if self.planning:
            return
        self._sync(Q, [in_], [out])
        ins = Q.eng.dma_start(out=out.ap, in_=in_.ap)
        dsem[1] += 16
        ins.then_inc(dsem[0], 16)
        self._mark(id(dsem[0]), dsem[1], [in_], [out])
        self.stats["dmas"] += 1

    def wait_all(self, E, tvs):
        if self.planning:
            return
        self._sync(E, tvs, [])

    def mm_group(self, out, pairs):
        reads = []
        for a, b in pairs:
            reads.append(a)
            reads.append(b)
        n = len(pairs)

        def fn():
            ins = None
            for i, (a, b) in enumerate(pairs):
                ins = self.nc.tensor.matmul(out.ap, lhsT=a.ap, rhs=b.ap, start=(i == 0), stop=(i == n - 1))
            return ins
        self.op(self.PE, fn, reads, [out])

    def act(self, out, in_, func, bias=None, scale=None, extra_reads=()):
        kw = {}
        if bias is not None:
            kw["bias"] = bias.ap if isinstance(bias, TV) else bias
        if scale is not None:
            kw["scale"] = scale.ap if isinstance(scale, TV) else scale
        reads = [in_] + [x for x in (bias, scale) if isinstance(x, TV)] + list(extra_reads)
        self.op(self.ACT, lambda: self.nc.scalar.activation(out=out.ap, in_=in_.ap, func=func, **kw), reads, [out])

    def tt(self, out, in0, in1, op, E=None):
        E = E or self.DVE
        self.op(E, lambda: E.eng.tensor_tensor(out=out.ap, in0=in0.ap, in1=in1.ap, op=op), [in0, in1], [out])

    def ts(self, out, in0, s1, s2, op0, op1=None, E=None):
        E = E or self.DVE
        reads = [in0] + [x for x in (s1, s2) if isinstance(x, TV)]
        a1 = s1.ap if isinstance(s1, TV) else s1
        a2 = s2.ap if isinstance(s2, TV) else s2
        if op1 is None:
            fn = lambda: E.eng.tensor_scalar(out=out.ap, in0=in0.ap, scalar1=a1, scalar2=None, op0=op0)
        else:
            fn = lambda: E.eng.tensor_scalar(out=out.ap, in0=in0.ap, scalar1=a1, scalar2=a2, op0=op0, op1=op1)
        self.op(E, fn, reads, [out])

    def stt(self, out, in0, scalar, in1, op0, op1, E=None):
        E = E or self.DVE
        reads = [in0, in1] + ([scalar] if isinstance(scalar, TV) else [])
        sc = scalar.ap if isinstance(scalar, TV) else scalar
        self.op(E, lambda: E.eng.scalar_tensor_tensor(out=out.ap, in0=in0.ap, scalar=sc, in1=in1.ap, op0=op0, op1=op1),
                reads, [out])

    def copy(self, out, in_, E=None):
        E = E or self.DVE
        self.op(E, lambda: E.eng.tensor_copy(out=out.ap, in_=in_.ap), [in_], [out])

    def recip(self, out, in_):
        self.op(self.DVE, lambda: self.nc.vector.reciprocal(out=out.ap, in_=in_.ap), [in_], [out])

    def memset(self, out, val, E=None):
        E = E or self.DVE
        self.op(E, lambda: E.eng.memset(out.ap, val), [], [out])


def LD(src, off, K, N, c0=0, n=None, P=128, q=None):
    return dict(src=src, off=off, K=K, N=N, c0=c0, n=(N if n is None else n), P=P, q=q)


class Stream:
    def __init__(self, k, name, nslots, slot_elems):
        self.k = k
        self.nslots = nslots
        self.slots = [Tile(k, f"{name}{i}", [128, slot_elems], BF16) for i in range(nslots)]
        self.dsems = [k.new_dsem(f"{name}{i}") for i in range(nslots)]
        self.dsems_hw = [k.new_dsem(f"{name}h{i}") for i in range(nslots)]
        self.slot_elems = slot_elems
        self.plan = []
        self.issued = 0
        self.consumed = 0

    def _view(self, tile, off, K, N, P=128):
        assert off + K * N <= self.slot_elems
        return TV(tile.t[0:P, off:off + K * N].rearrange("p (k n) -> p k n", k=K), [tile.buf], tile)

    def _issue(self, i):
        loads, views = self.plan[i]
        s = i % self.nslots
        tile = self.slots[s]
        for ld in loads:
            v = self._view(tile, ld["off"], ld["K"], ld["N"], ld["P"])
            dst = TV(v.ap[:, :, ld["c0"]:ld["c0"] + ld["n"]], [tile.buf], tile)
            sem = self.dsems[s] if ld["q"] is self.k.POOL else self.dsems_hw[s]
            self.k.dma(ld["q"], dst, ld["src"], sem)

    def idx(self):
        return len(self.plan) if self.k.planning else self.consumed

    def next(self, loads, views, cont=False, anchor=None):
        if self.k.planning:
            self.plan.append((loads, views))
            return [None for _ in views]
        i = self.consumed
        if anchor is not None:
            self.gfirst = anchor
        elif not cont:
            self.gfirst = i
        while self.issued < min(len(self.plan), self.gfirst + self.nslots):
            self._issue(self.issued)
            self.issued += 1
        self.consumed += 1
        tile = self.slots[i % self.nslots]
        return [self._view(tile, *v) for v in self.plan[i][1]]


CQ0, CKV0, KR0, CONV0, MQ0, MK0, MV0, G0 = 0, 768, 1280, 1344, 3392, 4416, 5440, 6464
DIN = 12608
NEG = -30000.0
STOP = None
MLA_SCALE = 192.0 ** -0.5
MOBA_SCALE = 128.0 ** -0.5

WSHAPES = {
    "ffn1_w_gate": [D, DFF], "ffn1_w_up": [D, DFF], "ffn1_w_down": [DFF, D],
    "ffn2_w_gate": [D, DFF], "ffn2_w_up": [D, DFF], "ffn2_w_down": [DFF, D],
    "w_in": [D, DIN], "mla_w_uq": [768, 1536], "mla_w_ukv": [512, 2048],
    "mla_w_o": [1024, D], "conv_w_pw": [1024, D], "moba_w_o": [1024, D], "w_out": [D, D],
}
SSHAPES = {
    "qa": ([8, 192, T], BF16), "ka": ([8, 192, T], BF16), "va": ([T, 1024], BF16),
    "qc": ([8, 128, T], BF16), "kc": ([8, 128, T], BF16), "vc": ([T, 1024], BF16),
    "zc": ([1024, T], F32), "gt": ([3, D, T], BF16),
}
REMOTE = ("ka", "va", "kc", "vc", "zc")


def vec_layout():
    off = {}
    o = 0
    for name, n in (("ffn1_norm", 16), ("mix_norm", 16), ("ffn2_norm", 16), ("b_gate", 48), ("cq_norm", 6),
                    ("ckv_norm", 4), ("qn_nope", 1), ("qn_rope", 1), ("qn_rperm", 1), ("kn_nope", 1),
                    ("kn_rope", 1), ("kn_rperm", 1), ("conv_w", 248), ("conv_b", 8), ("ln_g", 8), ("ln_b", 8),
                    ("mq_norm", 1), ("mk_norm", 1)):
        off[name] = o
        o += n
    off["_per_layer"] = o
    return off


VL = vec_layout()
NVEC = VL["_per_layer"] * DEPTH
CB_COLS = 2304
CS_COLS = 2 + 192
CBF_COLS = 128 + 1024


class Prog:
    def __init__(self, k, fused):
        self.k = k
        self.fused = fused
        self.dts = {}
        self.in_names = []
        self.out_names = []
        self.XT = [[Tile(k, f"xt{c}_{t}", [128, TT], F32) for t in range(NT)] for c in range(NCH)]
        self.HT = [[Tile(k, f"ht{c}_{t}", [128, TT], BF16) for t in range(NT)] for c in range(NCH)]
        self.pool = [Tile(k, f"pl{i}", [128, TT], BF16) for i in range(32)]
        self.ones = Tile(k, "ones", [128, 128], BF16)
        self.sq = [Tile(k, f"sq{i}", [128, TT], BF16) for i in range(2)]
        self.rstd = [Tile(k, f"rstd{i}", [128, TT], F32) for i in range(2)]
        self.tmpf = [Tile(k, f"tmpf{i}", [128, TT], F32) for i in range(4)]
        self.tmpi = 0
        self.stg = [Tile(k, f"stg{i}", [128, TT], BF16) for i in range(4)]
        self.stgsem = [k.new_dsem(f"stg{i}") for i in range(4)]
        self.stgi = 0
        self.tmpsem = [k.new_dsem(f"tmpf{i}") for i in range(4)]
        self.vecs = Tile(k, "vecs", [128, NVEC], F32)
        self.cbuf = Tile(k, "cbuf", [128, CB_COLS], F32)
        self.cbsem = k.new_dsem("cbuf")
        self.cs = Tile(k, "cs", [128, CS_COLS], F32)
        self.cbf = Tile(k, "cbf", [128, CBF_COLS], BF16)
        self.kr = Tile(k, "kr", [64, T], F32)
        self.ssr = Tile(k, "ssr", [128, T], F32)
        self.zb = self.ssr
        self.zbsem = k.new_dsem("zb")
        self.km = Tile(k, "km", [128, 16], F32)
        self.kmb = Tile(k, "kmb", [128, 32], BF16)
        self.gm = Tile(k, "gm", [128, 64], F32)
        self.cnt = Tile(k, "cnt", [128, 64], F32)
        self.cmp = Tile(k, "cmp", [128, 64], F32)
        self.pen = Tile(k, "pen", [128, 64], F32)
        self.penb = Tile(k, "penb", [128, 64], BF16)
        self.penT = [Tile(k, f"penT{i}", [8, T], BF16) for i in range(2)]
        self.wst = Stream(k, "w", 4, 4096)
        self.xsem = k.new_dsem("x")
        self.xssem = k.new_dsem("xs")
        self.csem = k.new_dsem("c")
        self.cssem = k.new_dsem("cs")
        self.v = 0

    def dram(self, name, shape, dtype, kind):
        if name not in self.dts:
            self.dts[name] = DTile(self.k, name, shape, dtype, kind)
            if kind == "ExternalInput":
                self.in_names.append(name)
            elif kind == "ExternalOutput":
                self.out_names.append(name)
        return self.dts[name]

    def W(self, name, l):
        return self.dram(f"{name}_{l}", WSHAPES[name], F32, "ExternalInput")

    def S(self, name, l, mode):
        shape, dt = SSHAPES[name]
        if self.fused:
            vv = 0 if mode == "rr" else self.v
            return self.dram(f"{name}_{l}_{vv}", shape, dt, "Internal")
        if mode == "w":
            return self.dram(f"{name}_{l}", shape, dt, "ExternalOutput")
        if mode == "r":
            return self.dram(f"{name}_{l}_own", shape, dt, "ExternalInput")
        return self.dram(f"{name}_{l}_rem", shape, dt, "ExternalInput")

    def tmp(self):
        i = self.tmpi
        self.tmpi = (self.tmpi + 1) % len(self.tmpf)
        return self.tmpf[i]

    def tmp_i(self):
        i = self.tmpi
        self.tmpi = (self.tmpi + 1) % len(self.tmpf)
        return i

    def stage(self):
        i = self.stgi
        self.stgi = (self.stgi + 1) % len(self.stg)
        return i

    def wload(self, w, r0, nrows, c0, ncols, off, c0d=0, Ntot=None):
        src = w.v(w.ap[r0:r0 + nrows, c0:c0 + ncols].rearrange("(k p) n -> p k n", p=128), "all")
        return LD(src, off, nrows // 128, Ntot or ncols, c0d, ncols, 128, self.k.POOL)

    def setup(self):
        k = self.k
        k.memset(self.ones[:], 1.0)
        vin = self.dram("vecs_in", [128, NVEC], F32, "ExternalInput")
        cbfin = self.dram("cbf_in", [128, CBF_COLS], BF16, "ExternalInput")
        k.dma(k.SP, self.vecs[:], vin.v(vin.ap, "all"), self.csem)
        k.dma(k.SP, self.cbf[:], cbfin.v(cbfin.ap, "all"), self.csem)
        if not self.fused:
            csin = self.dram("cs_in", [128, CS_COLS], F32, "ExternalInput")
            k.dma(k.SP, self.cs[:], csin.v(csin.ap, "all"), self.csem)
        if not k.planning:
            tot = (id(self.csem[0]), self.csem[1])
            for tl in (self.vecs, self.cbf) + (() if self.fused else (self.cs,)):
                tl.buf.writer = tot

    def set_half(self, v):
        self.v = v
        csin = self.dram("cs_in", [2, 128, CS_COLS], F32, "ExternalInput")
        self.k.dma(self.k.SP, self.cs[:], csin.v(csin.ap[v], "all"), self.cssem)

    def load_cbuf(self, which):
        k = self.k
        if which == "rope":
            if self.fused:
                src = self.dram("rope_in", [2, 64, 2 * T], F32, "ExternalInput")
                k.dma(k.SP, self.cbuf[0:64, 0:2 * T], src.v(src.ap[self.v], "all"), self.cbsem)
            else:
                src = self.dram("rope_in", [64, 2 * T], F32, "ExternalInput")
                k.dma(k.SP, self.cbuf[0:64, 0:2 * T], src.v(src.ap, "all"), self.cbsem)
        else:
            src = self.dram("mask_in", [128, CB_COLS], F32, "ExternalInput")
            k.dma(k.SP, self.cbuf[:], src.v(src.ap, "all"), self.cbsem)

    def load_x(self):
        k = self.k
        xT = self.dram("xT", [D, T], F32, "ExternalInput")
        for c in range(NCH):
            for t in range(NT):
                k.dma(k.SP, self.XT[c][t][:], xT.v(xT.ap[c * 128:(c + 1) * 128, t * TT:(t + 1) * TT], "all"), self.xsem)
        if not k.planning:
            tot = (id(self.xsem[0]), self.xsem[1])
            for c in range(NCH):
                for t in range(NT):
                    self.XT[c][t].buf.writer = tot

    def load_xs(self, first):
        k = self.k
        src = self.dram("xT", [2, D, T], F32, "ExternalInput") if first else self.dram("xs", [2, D, T], F32, "Internal")
        v = self.v
        for c in range(NCH):
            for t in range(NT):
                k.dma(k.SP, self.XT[c][t][:], src.v(src.ap[v, c * 128:(c + 1) * 128, t * TT:(t + 1) * TT], (v, c, t)), self.xsem)
        if not k.planning:
            sid = id(self.xsem[0])
            tot = (sid, self.xsem[1])
            for c in range(NCH):
                for t in range(NT):
                    self.XT[c][t].buf.writer = tot
                    b = src.bufs[(v, c, t)]
                    b.readers = [(s_, v_) if s_ != sid else tot for (s_, v_) in b.readers]

    def store_xs(self, last):
        k = self.k
        dst = self.dram("yT", [2, D, T], F32, "ExternalOutput") if last else self.dram("xs", [2, D, T], F32, "Internal")
        v = self.v
        for c in range(NCH):
            for t in range(NT):
                k.dma(k.SP, dst.v(dst.ap[v, c * 128:(c + 1) * 128, t * TT:(t + 1) * TT], (v, c, t)), self.XT[c][t][:], self.xssem)
        if not k.planning:
            sid = id(self.xssem[0])
            tot = (sid, self.xssem[1])
            for c in range(NCH):
                for t in range(NT):
                    b = self.XT[c][t].buf
                    b.readers = [(s_, v_) if s_ != sid else tot for (s_, v_) in b.readers]
                    dst.bufs[(v, c, t)].writer = tot

    def store_x(self):
        k = self.k
        yT = self.dram("yT", [D, T], F32, "ExternalOutput")
        for c in range(NCH):
            for t in range(NT):
                k.dma(k.SP, yT.v(yT.ap[c * 128:(c + 1) * 128, t * TT:(t + 1) * TT], (c, t)), self.XT[c][t][:], self.xsem)

    def finish(self):
        k = self.k
        if k.planning:
            return
        for (sem, cnt) in [self.xsem, self.xssem] + self.stgsem + self.tmpsem:
            if cnt > 0:
                k.SP.eng.wait_ge(sem, cnt)

    def rstd_from(self, ss, dim, out, P=128):
        k = self.k
        tm = self.tmp()
        k.act(tm[0:P, :], ss, AF.Sqrt, bias=EPS, scale=1.0 / dim)
        k.recip(out, tm[0:P, :])

    def ones_mm(self, ps, pairs):
        k = self.k
        k.mm_group(ps, [(self.ones[0:P, :], src) for (src, P) in pairs])

    def store(self, dst, si, view=None):
        k = self.k
        src = self.stg[si][:] if view is None else view
        k.dma(k.SP, dst, src, self.stgsem[si])

    def rmsnorm_x(self, gcol):
        k = self.k
        k.set_rr(6)
        for t in range(NT):
            ps = k.psum[6 + t]
            for c in range(NCH):
                sq = self.sq[c % 2]
                k.act(sq[:], self.XT[c][t][:], AF.Square)
                k.op(k.PE, (lambda c=c, sq=sq, ps=ps: k.nc.tensor.matmul(ps.t[:], lhsT=self.ones.t[:], rhs=sq.t[:],
                                                                          start=(c == 0), stop=(c == NCH - 1))),
                     [self.ones[:], sq[:]], [ps[:]])
            r = self.rstd[t]
            self.rstd_from(ps[:], D, r[:])
            for c in range(NCH):
                k.stt(self.HT[c][t][:], self.XT[c][t][:], self.vecs[:, gcol + c:gcol + c + 1], r[:], ALU.mult, ALU.mult)

    def mm2(self, pss, wt, kchunks, cols, rhs):
        k = self.k
        reads = [wt] + [rhs[c][t][:] for c in range(kchunks) for t in range(NT)]

        def fn():
            ins = None
            for c in range(kchunks):
                for t in range(NT):
                    ins = k.nc.tensor.matmul(pss[t].ap, lhsT=wt.ap[:, c, cols[0]:cols[1]], rhs=rhs[c][t].t[:],
                                             start=(c == 0), stop=(c == kchunks - 1))
            return ins
        k.op(k.PE, fn, reads, list(pss))

    def ffn(self, l, which):
        k = self.k
        wg, wu, wd = self.W(which + "_w_gate", l), self.W(which + "_w_up", l), self.W(which + "_w_down", l)
        gcol = l * VL["_per_layer"] + VL[which + "_norm"]
        self.rmsnorm_x(gcol)
        k.set_rr(8)
        actT = [[self.pool[j * NT + t] for t in range(NT)] for j in range(11)]
        QCH = FCH // 4
        for q in range(4):
            for jj in range(QCH):
                j = q * QCH + jj
                res = self.wst.next([self.wload(wg, 0, D, j * 128, 128, 0), self.wload(wu, 0, D, j * 128, 128, 2048)],
                                    [(0, NCH, 128, 128), (2048, NCH, 128, 128)])
                if k.planning:
                    continue
                wgt, wut = res
                psg = [k.ps() for _ in range(NT)]
                psu = [k.ps() for _ in range(NT)]
                self.mm2([p[:] for p in psg], wgt, NCH, (0, 128), self.HT)
                self.mm2([p[:] for p in psu], wut, NCH, (0, 128), self.HT)
                for t in range(NT):
                    tm = self.tmp()
                    k.act(tm[:], psg[t][:], AF.Silu)
                    k.tt(actT[jj][t][:], tm[:], psu[t][:], ALU.mult)
            for db in range(D // 256):
                res = self.wst.next([self.wload(wd, q * QCH * 128, QCH * 128, db * 256, 256, 0)], [(0, QCH, 256, 128)])
                if k.planning:
                    continue
                (wdt,) = res
                for dd in range(2):
                    d = db * 2 + dd
                    pss = [k.ps() for _ in range(NT)]
                    self.mm2([p[:] for p in pss], wdt, QCH, (dd * 128, dd * 128 + 128), actT)
                    for t in range(NT):
                        x = self.XT[d][t]
                        k.stt(x[:], pss[t][:], 0.5, x[:], ALU.mult, ALU.add)

    def mixer_in(self, l):
        k = self.k
        VB = l * VL["_per_layer"]
        vc = lambda name, i=0, P=128: self.vecs[0:P, VB + VL[name] + i:VB + VL[name] + i + 1]
        self.rmsnorm_x(VB + VL["mix_norm"])
        k.set_rr(6)
        self.load_cbuf("rope")
        if STOP == 'setup':
            return
        w_in = self.W("w_in", l)
        w_uq = self.W("mla_w_uq", l)
        w_ukv = self.W("mla_w_ukv", l)
        qa, ka, va = self.S("qa", l, "w"), self.S("ka", l, "w"), self.S("va", l, "w")
        qc, kc, vcd = self.S("qc", l, "w"), self.S("kc", l, "w"), self.S("vc", l, "w")
        zc, gt = self.S("zc", l, "w"), self.S("gt", l, "w")
        cq = [[self.pool[i * NT + t] for t in range(NT)] for i in range(6)]
        ckv = [[self.pool[12 + i * NT + t] for t in range(NT)] for i in range(4)]
        tsl = lambda t: slice(t * TT, (t + 1) * TT)

        def latent(col0, nch, dst, gname, dim):
            ss = [k.psum[6], k.psum[7]]
            for b in range(0, nch, 2):
                res = self.wst.next([self.wload(w_in, 0, D, col0 + b * 128, 256, 0)], [(0, NCH, 256, 128)])
                if k.planning:
                    continue
                (wt,) = res
                for i in range(2):
                    ci = b + i
                    pss = [k.ps() for _ in range(NT)]
                    self.mm2([p[:] for p in pss], wt, NCH, (i * 128, i * 128 + 128), self.HT)
                    for t in range(NT):
                        sq = self.sq[t]
                        k.act(sq[:], pss[t][:], AF.Square)
                        k.copy(dst[ci][t][:], pss[t][:])
                        k.op(k.PE, (lambda t=t, sq=sq, ci=ci: k.nc.tensor.matmul(
                            ss[t].t[:], lhsT=self.ones.t[:], rhs=sq.t[:], start=(ci == 0), stop=(ci == nch - 1))),
                            [self.ones[:], sq[:]], [ss[t][:]])
            if k.planning:
                return
            for t in range(NT):
                self.rstd_from(ss[t][:], dim, self.rstd[t][:])
                for i in range(nch):
                    k.stt(dst[i][t][:], dst[i][t][:], vc(gname, i), self.rstd[t][:], ALU.mult, ALU.mult)

        latent(CQ0, 6, cq, "cq_norm", 768.0)
        latent(CKV0, 4, ckv, "ckv_norm", 512.0)
        if STOP == 'latent':
            return

        res = self.wst.next([self.wload(w_in, 0, D, KR0, 64, 0),
                             self.wload(w_in, 0, D, KR0 + 32, 32, 1024, 0, 64),
                             self.wload(w_in, 0, D, KR0, 32, 1024, 32, 64)],
                            [(0, NCH, 64, 128), (1024, NCH, 64, 128)])
        if not k.planning:
            wa, wb = res
            psa = [k.ps() for _ in range(NT)]
            psb = [k.ps() for _ in range(NT)]
            self.mm2([p[0:64, :] for p in psa], wa, NCH, (0, 64), self.HT)
            self.mm2([p[0:64, :] for p in psb], wb, NCH, (0, 64), self.HT)
            for t in range(NT):
                sq = self.sq[t]
                k.act(sq[0:64, :], psa[t][0:64, :], AF.Square)
                pss = k.ps()
                self.ones_mm(pss[:], [(sq[0:64, :], 64)])
                k.copy(self.ssr[:, tsl(t)], pss[:])
                t1, t2 = self.tmp(), self.tmp()
                k.stt(t1[0:64, :], psa[t][0:64, :], vc("kn_rope", 0, 64), self.cbuf[0:64, tsl(t)], ALU.mult, ALU.mult)
                k.stt(t2[0:64, :], psb[t][0:64, :], vc("kn_rperm", 0, 64), self.cbuf[0:64, T + t * TT:T + (t + 1) * TT],
                      ALU.mult, ALU.mult)
                k.tt(self.kr[0:64, tsl(t)], t1[0:64, :], t2[0:64, :], ALU.add)

        if STOP == 'kr':
            return
        for h in range(8):
            c0 = h * 192
            res = self.wst.next([self.wload(w_uq, 0, 768, c0, 128, 0),
                                 self.wload(w_uq, 0, 768, c0 + 128, 64, 768),
                                 self.wload(w_uq, 0, 768, c0 + 160, 32, 1152, 0, 64),
                                 self.wload(w_uq, 0, 768, c0 + 128, 32, 1152, 32, 64)],
                                [(0, 6, 128, 128), (768, 6, 64, 128), (1152, 6, 64, 128)])
            if k.planning:
                continue
            wn, wr, ws = res
            psn = [k.ps() for _ in range(NT)]
            self.mm2([p[:] for p in psn], wn, 6, (0, 128), cq)
            psr = [k.ps() for _ in range(NT)]
            self.mm2([p[0:64, :] for p in psr], wr, 6, (0, 64), cq)
            pss_ = [k.ps() for _ in range(NT)]
            self.mm2([p[0:64, :] for p in pss_], ws, 6, (0, 64), cq)
            for t in range(NT):
                k.act(self.sq[0][:], psn[t][:], AF.Square)
                k.act(self.sq[1][0:64, :], psr[t][0:64, :], AF.Square)
                pst = k.psum[6 + t]
                self.ones_mm(pst[:], [(self.sq[0][:], 128), (self.sq[1][0:64, :], 64)])
                r = self.tmp()
                self.rstd_from(pst[:], 192.0, r[:])
                si = self.stage()
                k.stt(self.stg[si][:], psn[t][:], vc("qn_nope"), r[:], ALU.mult, ALU.mult)
                self.store(qa.v(qa.ap[h, 0:128, tsl(t)], (h, t, 0)), si)
                t1, t2 = self.tmp(), self.tmp()
                k.stt(t1[0:64, :], psr[t][0:64, :], vc("qn_rope", 0, 64), self.cbuf[0:64, tsl(t)], ALU.mult, ALU.mult)
                k.stt(t2[0:64, :], pss_[t][0:64, :], vc("qn_rperm", 0, 64), self.cbuf[0:64, T + t * TT:T + (t + 1) * TT],
                      ALU.mult, ALU.mult)
                k.tt(t1[0:64, :], t1[0:64, :], t2[0:64, :], ALU.add)
                si = self.stage()
                k.tt(self.stg[si][0:64, :], t1[0:64, :], r[0:64, :], ALU.mult)
                self.store(qa.v(qa.ap[h, 128:192, tsl(t)], (h, t, 1)), si, self.stg[si][0:64, :])

        if STOP == 'q':
            return
        for h in range(8):
            res = self.wst.next([self.wload(w_ukv, 0, 512, h * 256, 128, 0)], [(0, 4, 128, 128)])
            if k.planning:
                continue
            (wk,) = res
            psk = [k.ps() for _ in range(NT)]
            self.mm2([p[:] for p in psk], wk, 4, (0, 128), ckv)
            for t in range(NT):
                k.act(self.sq[t][:], psk[t][:], AF.Square)
                pst = k.psum[6 + t]
                self.ones_mm(pst[:], [(self.sq[t][:], 128)])
                tot = self.tmp()
                k.tt(tot[:], pst[:], self.ssr[:, tsl(t)], ALU.add)
                r = self.tmp()
                self.rstd_from(tot[:], 192.0, r[:])
                si = self.stage()
                k.stt(self.stg[si][:], psk[t][:], vc("kn_nope"), r[:], ALU.mult, ALU.mult)
                self.store(ka.v(ka.ap[h, 0:128, tsl(t)], (h, t, 0)), si)
                si = self.stage()
                k.tt(self.stg[si][0:64, :], self.kr[0:64, tsl(t)], r[0:64, :], ALU.mult)
                self.store(ka.v(ka.ap[h, 128:192, tsl(t)], (h, t, 1)), si, self.stg[si][0:64, :])

        if STOP == 'k':
            return
        for g in range(2):
            res = self.wst.next([self.wload(w_ukv, 0, 512, (4 * g + hh) * 256 + 128, 128, 0, hh * 128, 512) for hh in range(4)],
                                [(0, 4, 512, 128)])
            if k.planning:
                continue
            (wv,) = res
            for t in range(NT):
                for sb in range(4):
                    ps = k.ps()
                    k.mm_group(ps[:], [(TV(ckv[i][t].t[:, sb * 128:(sb + 1) * 128], [ckv[i][t].buf]),
                                        TV(wv.ap[:, i, :], wv.bufs)) for i in range(4)])
                    si = self.stage()
                    k.copy(self.stg[si][:], ps[:])
                    r0 = t * TT + sb * 128
                    self.store(va.v(va.ap[r0:r0 + 128, g * 512:(g + 1) * 512], (r0, g)), si)

        if STOP == 'v':
            return
        for (col0, dst, gname) in ((MQ0, qc, "mq_norm"), (MK0, kc, "mk_norm")):
            for hb in range(4):
                res = self.wst.next([self.wload(w_in, 0, D, col0 + hb * 256, 256, 0)], [(0, NCH, 256, 128)])
                if k.planning:
                    continue
                (wt,) = res
                for i in range(2):
                    h = hb * 2 + i
                    pss = [k.ps() for _ in range(NT)]
                    self.mm2([p[:] for p in pss], wt, NCH, (i * 128, i * 128 + 128), self.HT)
                    for t in range(NT):
                        k.act(self.sq[t][:], pss[t][:], AF.Square)
                        pst = k.psum[6 + t]
                        self.ones_mm(pst[:], [(self.sq[t][:], 128)])
                        r = self.tmp()
                        self.rstd_from(pst[:], 128.0, r[:])
                        si = self.stage()
                        k.stt(self.stg[si][:], pss[t][:], vc(gname), r[:], ALU.mult, ALU.mult)
                        self.store(dst.v(dst.ap[h, :, tsl(t)], (h, t)), si)

        if STOP == 'mqk':
            return
        for blk in range(4):
            res = self.wst.next([self.wload(w_in, 0, D, MV0 + blk * 256, 256, 0)], [(0, NCH, 256, 128)])
            if k.planning:
                continue
            (wt,) = res
            for t in range(NT):
                for sb in range(4):
                    ps = k.ps()
                    k.mm_group(ps[:, 0:256], [(TV(self.HT[c][t].t[:, sb * 128:(sb + 1) * 128], [self.HT[c][t].buf]),
                                               TV(wt.ap[:, c, :], wt.bufs)) for c in range(NCH)])
                    si = self.stage()
                    k.copy(self.stg[si][:, 0:256], ps[:, 0:256])
                    r0 = t * TT + sb * 128
                    self.store(vcd.v(vcd.ap[r0:r0 + 128, blk * 256:(blk + 1) * 256], (r0, blk)), si, self.stg[si][:, 0:256])

        if STOP == 'mv':
            return
        for i in range(8):
            res = self.wst.next([self.wload(w_in, 0, D, CONV0 + i * 128, 128, 0),
                                 self.wload(w_in, 0, D, CONV0 + 1024 + i * 128, 128, 2048)],
                                [(0, NCH, 128, 128), (2048, NCH, 128, 128)])
            if k.planning:
                continue
            wa, wg_ = res
            psa = [k.ps() for _ in range(NT)]
            psg = [k.ps() for _ in range(NT)]
            self.mm2([p[:] for p in psa], wa, NCH, (0, 128), self.HT)
            self.mm2([p[:] for p in psg], wg_, NCH, (0, 128), self.HT)
            for t in range(NT):
                sg = self.tmp()
                k.act(sg[:], psg[t][:], AF.Sigmoid)
                zi = self.tmp_i()
                k.tt(self.tmpf[zi][:], sg[:], psa[t][:], ALU.mult)
                k.dma(k.SP, zc.v(zc.ap[i * 128:(i + 1) * 128, tsl(t)], (i, t)), self.tmpf[zi][:], self.tmpsem[zi])

        if STOP == 'conv':
            return
        for b in range(3):
            for cb in range(8):
                res = self.wst.next([self.wload(w_in, 0, D, G0 + b * D + cb * 256, 256, 0)], [(0, NCH, 256, 128)])
                if k.planning:
                    continue
                (wt,) = res
                for i in range(2):
                    c = cb * 2 + i
                    pss = [k.ps() for _ in range(NT)]
                    self.mm2([p[:] for p in pss], wt, NCH, (i * 128, i * 128 + 128), self.HT)
                    for t in range(NT):
                        si = self.stage()
                        k.act(self.stg[si][:], pss[t][:], AF.Sigmoid, bias=vc("b_gate", b * 16 + c))
                        self.store(gt.v(gt.ap[b, c * 128:(c + 1) * 128, tsl(t)], (b, c, t)), si)

    def attn_tiles(self, h, t, moba, Kn, Kr, Vv, Qn, Qr, penT, dst):
        k = self.k
        acc_o = k.psum[4 + 2 * (t % 2)]
        acc_d = k.psum[5 + 2 * (t % 2)]
        blocks = [("r", 0)] + [("l", j) for j in range(4 * t, 4 * t + 4)] + [("r", kb) for kb in range(1, 8)] + \
                 [("l", j) for j in range(0, 4 * t)]
        slope = 2.0 ** (-(h + 1))
        nblk = len(blocks)
        LA = 3
        pend = []

        def score(bi, kind, j):
            kcol = j * 128 if kind == "r" else T + j * 128
            vblk = j if kind == "r" else 8 + j
            r = (j - 4 * t) if (kind == "l" and j >= 4 * t) else None
            q0 = 128 * r if r is not None else 0
            qs = slice(t * TT + q0, (t + 1) * TT)
            ps = k.ps()
            pairs = [(TV(Kn.ap[:, 0, kcol:kcol + 128], Kn.bufs), TV(Qn.ap[:, 0, qs], Qn.bufs))]
            if not moba:
                pairs.append((TV(Kr.ap[0:64, 0, kcol:kcol + 128], Kr.bufs), TV(Qr.ap[0:64, 0, qs], Qr.bufs)))
            else:
                slot = (j // 2) if kind == "r" else 4 + j // 2
                pairs.append((self.cbf[0:8, 128 + slot * 128:128 + (slot + 1) * 128], penT[0:8, qs]))
            k.mm_group(ps[:, q0:TT], pairs)
            pi = self.stage()
            pt = self.stg[pi]
            if not moba:
                if r is None:
                    bias = self.cs[:, 0:1] if kind == "r" else None
                    k.act(pt[:, q0:TT], ps[:, q0:TT], AF.Exp, bias=bias, scale=MLA_SCALE)
                else:
                    tm = self.tmp()
                    w0 = (3 - r) * 128
                    k.tt(tm[:, q0:TT], ps[:, q0:TT], self.cbuf[:, w0 + q0:w0 + TT], ALU.add)
                    k.act(pt[:, q0:TT], tm[:, q0:TT], AF.Exp, scale=MLA_SCALE)
            else:
                tm = self.tmp()
                if r is None:
                    delta = ((T + t * TT) - j * 128) if kind == "r" else (t * TT - j * 128)
                    k.stt(tm[:, :], self.cbuf[:, 1792:2304], slope / MOBA_SCALE, ps[:, :], ALU.mult, ALU.add)
                    k.act(pt[:, :], tm[:, :], AF.Exp, bias=-slope * delta, scale=MOBA_SCALE)
                else:
                    w0 = 896 + (3 - r) * 128
                    k.stt(tm[:, q0:TT], self.cbuf[:, w0 + q0:w0 + TT], slope / MOBA_SCALE, ps[:, q0:TT], ALU.mult, ALU.add)
                    k.act(pt[:, q0:TT], tm[:, q0:TT], AF.Exp, scale=MOBA_SCALE)
            return (bi, vblk, pt, q0)

        def accum(bi, vblk, pt, q0):
            first, last = (bi == 0), (bi == nblk - 1)
            k.op(k.PE, (lambda: k.nc.tensor.matmul(acc_o.t[:, q0:TT], lhsT=Vv.ap[:, vblk, :], rhs=pt.t[:, q0:TT],
                                                   start=first, stop=last)), [Vv, pt[:]], [acc_o[:]])
            k.op(k.PE, (lambda: k.nc.tensor.matmul(acc_d.t[:, q0:TT], lhsT=self.ones.t[:], rhs=pt.t[:, q0:TT],
                                                   start=first, stop=last)), [self.ones[:], pt[:]], [acc_d[:]])

        for bi, (kind, j) in enumerate(blocks):
            pend.append(score(bi, kind, j))
            if len(pend) > LA:
                accum(*pend.pop(0))
        while pend:
            accum(*pend.pop(0))
        rec = self.tmp()
        k.recip(rec[:], acc_d[:])
        k.tt(dst[:], acc_o[:], rec[:], ALU.mult)

    def moba_select(self, Kn, Qn, penT):
        k = self.k
        km, kmb = self.km, self.kmb
        k.op(k.DVE, lambda: k.nc.vector.tensor_reduce(out=km.t[:, 0:8], in_=Kn.ap[:, 0, :].rearrange("p (s n) -> p s n", s=8),
                                                       axis=AX.X, op=ALU.add), [Kn], [km[:]])
        k.ts(kmb[:, 0:8], km[:, 0:8], 1.0 / 256.0, None, ALU.mult)
        ps = k.ps()

        def gates():
            ins = None
            for s_ in range(8):
                ins = k.nc.tensor.matmul(ps.t[:, s_ * 8:(s_ + 1) * 8], lhsT=Qn.ap[:, 0, s_ * 128:(s_ + 1) * 128],
                                         rhs=kmb.t[:, 0:8], start=True, stop=True)
            return ins
        k.op(k.PE, gates, [Qn, kmb[:]], [ps[:]])
        gm, cnt, cmp_, pen, penb = self.gm, self.cnt, self.cmp, self.pen, self.penb
        k.tt(gm[:], ps[:, 0:64], self.cs[:, 2:66], ALU.add)
        gm3 = gm.t[:].rearrange("p (s n) -> p s n", s=8)
        for m in range(8):
            bc = TV(gm3[:, :, m:m + 1].to_broadcast([128, 8, 8]), [gm.buf])
            dst = cnt if m == 0 else cmp_
            k.tt(TV(dst.t[:].rearrange("p (s n) -> p s n", s=8), [dst.buf]), TV(gm3, [gm.buf]), bc, ALU.is_lt)
            if m > 0:
                k.tt(cnt[:], cnt[:], cmp_[:], ALU.add)
        k.tt(cnt[:], cnt[:], self.cs[:, 130:194], ALU.mult)
        k.ts(pen[:], cnt[:], 3.0, NEG, ALU.is_ge, ALU.mult)
        k.tt(penb[:], pen[:], self.cs[:, 66:130], ALU.add)
        for hf in range(2):
            ps2 = k.ps()

            def tr(hf=hf, ps2=ps2):
                ins = None
                for s4 in range(4):
                    s_ = hf * 4 + s4
                    ins = k.nc.tensor.matmul(ps2.t[0:8, s4 * 128:(s4 + 1) * 128], lhsT=penb.t[:, s_ * 8:(s_ + 1) * 8],
                                             rhs=self.cbf.t[:, 0:128], start=True, stop=True)
                return ins
            k.op(k.PE, tr, [penb[:], self.cbf[:, 0:128]], [ps2[0:8, :]])
            k.copy(penT[0:8, hf * TT:(hf + 1) * TT], ps2[0:8, :])

    def conv_chunk(self, l, i, ZC):
        k = self.k
        VB = l * VL["_per_layer"]
        zc = self.S("zc", l, "r")
        zr = self.S("zc", l, "rr")
        zb = self.zb
        wcol = VB + VL["conv_w"] + i * 31
        bcol = VB + VL["conv_b"] + i
        for t in range(NT):
            if t == 0:
                k.dma(k.SP, zb[:, 30:30 + TT], zc.v(zc.ap[i * 128:(i + 1) * 128, 0:TT], (i, 0)), self.zbsem)
                k.dma(k.SP, zb[:, 0:30], zr.v(zr.ap[i * 128:(i + 1) * 128, T - 30:T], (i, NT - 1)), self.zbsem)
                k.ts(zb[:, 0:30], zb[:, 0:30], self.cs[:, 1:2], None, ALU.mult)
            else:
                k.dma(k.SP, zb[:, 0:30 + TT], zc.v(zc.ap[i * 128:(i + 1) * 128, t * TT - 30:(t + 1) * TT], (i, t - 1), (i, t)),
                      self.zbsem)
            accA, accB = self.tmp(), self.tmp()
            k.ts(accA[:], zb[:, 0:TT], self.vecs[:, wcol:wcol + 1], self.vecs[:, bcol:bcol + 1], ALU.mult, ALU.add)
            k.ts(accB[:], zb[:, 1:1 + TT], self.vecs[:, wcol + 1:wcol + 2], None, ALU.mult)
            for tap in range(2, 31):
                acc = accA if tap % 2 == 0 else accB
                k.stt(acc[:], zb[:, tap:tap + TT], self.vecs[:, wcol + tap:wcol + tap + 1], acc[:], ALU.mult, ALU.add)
            k.tt(ZC[i][t][:], accA[:], accB[:], ALU.add)

    def conv_finish(self, l, ZC):
        k = self.k
        VB = l * VL["_per_layer"]
        for t in range(NT):
            s1, s2 = k.psum[4], k.psum[5]
            for i in range(8):
                k.act(self.sq[i % 2][:], ZC[i][t][:], AF.Square)
                k.op(k.PE, (lambda i=i: k.nc.tensor.matmul(s1.t[:], lhsT=self.ones.t[:], rhs=ZC[i][t].t[:],
                                                            start=(i == 0), stop=(i == 7))), [self.ones[:], ZC[i][t][:]], [s1[:]])
                k.op(k.PE, (lambda i=i: k.nc.tensor.matmul(s2.t[:], lhsT=self.ones.t[:], rhs=self.sq[i % 2].t[:],
                                                            start=(i == 0), stop=(i == 7))), [self.ones[:], self.sq[i % 2][:]], [s2[:]])
            mean, var = self.rstd[0], self.rstd[1]
            k.ts(mean[:], s1[:], 1.0 / 1024.0, None, ALU.mult)
            msq = self.tmp()
            k.tt(msq[:], mean[:], mean[:], ALU.mult)
            k.stt(var[:], s2[:], 1.0 / 1024.0, msq[:], ALU.mult, ALU.subtract)
            self.rstd_from(var[:], 1.0, var[:])
            for i in range(8):
                tm = self.tmp()
                k.tt(tm[:], ZC[i][t][:], mean[:], ALU.subtract)
                k.tt(tm[:], tm[:], var[:], ALU.mult)
                k.act(ZC[i][t][:], tm[:], AF.Silu, bias=self.vecs[:, VB + VL["ln_b"] + i:VB + VL["ln_b"] + i + 1],
                      scale=self.vecs[:, VB + VL["ln_g"] + i:VB + VL["ln_g"] + i + 1])

    def mixer_out(self, l):
        k = self.k
        k.set_rr(4)
        self.load_cbuf("mask")
        OA = [[self.HT[h][t] for t in range(NT)] for h in range(8)]
        OC = [[self.HT[8 + h][t] for t in range(NT)] for h in range(8)]
        ZC = [[self.pool[i * NT + t] for t in range(NT)] for i in range(8)]
        qa, ka, va = self.S("qa", l, "r"), self.S("ka", l, "r"), self.S("va", l, "r")
        kar, var_ = self.S("ka", l, "rr"), self.S("va", l, "rr")
        qc, kc, vcd = self.S("qc", l, "r"), self.S("kc", l, "r"), self.S("vc", l, "r")
        kcr, vcr = self.S("kc", l, "rr"), self.S("vc", l, "rr")
        SPq = k.SP
        allk = lambda d, h, parts: [(h, t, p) for t in range(NT) for p in parts]
        def head_loads(u):
            moba = u >= 8
            h = u % 8
            if not moba:
                l1 = [LD(kar.v(kar.ap[h, 0:128, :].rearrange("(o p) n -> p o n", o=1), *allk(ka, h, (0,))), 0, 1, T, q=SPq),
                      LD(ka.v(ka.ap[h, 0:128, :].rearrange("(o p) n -> p o n", o=1), *allk(ka, h, (0,))), T, 1, T, q=SPq),
                      LD(kar.v(kar.ap[h, 128:192, :].rearrange("(o p) n -> p o n", o=1), *allk(ka, h, (1,))), 2 * T, 1, T, P=64, q=SPq),
                      LD(ka.v(ka.ap[h, 128:192, :].rearrange("(o p) n -> p o n", o=1), *allk(ka, h, (1,))), 3 * T, 1, T, P=64, q=SPq)]
                v1 = [(0, 1, 2 * T, 128), (2 * T, 1, 2 * T, 64)]
                vkeys = [(r0, g) for r0 in range(0, T, 128) for g in range(2)]
                l2 = [LD(var_.v(var_.ap[:, h * 128:(h + 1) * 128].rearrange("(b p) d -> p b d", p=128), *vkeys), 0, 8, 128, q=SPq),
                      LD(va.v(va.ap[:, h * 128:(h + 1) * 128].rearrange("(b p) d -> p b d", p=128), *vkeys), 1024, 8, 128, q=SPq),
                      LD(qa.v(qa.ap[h, 0:128, :].rearrange("(o p) n -> p o n", o=1), *allk(qa, h, (0,))), 2048, 1, T, q=SPq),
                      LD(qa.v(qa.ap[h, 128:192, :].rearrange("(o p) n -> p o n", o=1), *allk(qa, h, (1,))), 3072, 1, T, P=64, q=SPq)]
                v2 = [(0, 16, 128, 128), (2048, 1, T, 128), (3072, 1, T, 64)]
            else:
                hk = [(h, t) for t in range(NT)]
                l1 = [LD(kcr.v(kcr.ap[h, :, :].rearrange("(o p) n -> p o n", o=1), *hk), 0, 1, T, q=SPq),
                      LD(kc.v(kc.ap[h, :, :].rearrange("(o p) n -> p o n", o=1), *hk), T, 1, T, q=SPq)]
                v1 = [(0, 1, 2 * T, 128)]
                vkeys = [(r0, b) for r0 in range(0, T, 128) for b in range(4)]
                l2 = [LD(vcr.v(vcr.ap[:, h * 128:(h + 1) * 128].rearrange("(b p) d -> p b d", p=128), *vkeys), 0, 8, 128, q=SPq),
                      LD(vcd.v(vcd.ap[:, h * 128:(h + 1) * 128].rearrange("(b p) d -> p b d", p=128), *vkeys), 1024, 8, 128, q=SPq),
                      LD(qc.v(qc.ap[h, :, :].rearrange("(o p) n -> p o n", o=1), *hk), 2048, 1, T, q=SPq)]
                v2 = [(0, 16, 128, 128), (2048, 1, T, 128)]
            return l1, v1, l2, v2

        for h in range(8):
            l1, v1, l2, v2 = head_loads(h)
            r1 = self.wst.next(l1, v1)
            r2 = self.wst.next(l2, v2, cont=True)
            if not k.planning:
                Kn, Kr = r1
                Vv, Qn, Qr = r2
                for t in range(NT):
                    self.attn_tiles(h, t, False, Kn, Kr, Vv, Qn, Qr, None, OA[h][t])
            if h % 2 == 1:
                self.conv_chunk(l, h // 2, ZC)
        cur = None
        for h in range(8):
            if cur is None:
                i0 = self.wst.idx()
                l1, v1, l2, v2 = head_loads(8 + h)
                r1 = self.wst.next(l1, v1)
                r2 = self.wst.next(l2, v2, cont=True)
                cur = (i0, r1, r2, self.penT[h % 2])
                if not k.planning:
                    self.moba_select(r1[0], r2[1], self.penT[h % 2])
            i0, r1, r2, pT = cur
            nxt = None
            if h + 1 < 8:
                i1 = self.wst.idx()
                l1, v1, l2, v2 = head_loads(8 + h + 1)
                n1 = self.wst.next(l1, v1, anchor=i0)
                n2 = self.wst.next(l2, v2, anchor=i0)
                nxt = (i1, n1, n2, self.penT[(h + 1) % 2])
            if not k.planning:
                (Kn,) = r1
                Vv, Qn = r2
                self.attn_tiles(h, 0, True, Kn, None, Vv, Qn, None, pT, OC[h][0])
                if nxt is not None:
                    self.moba_select(nxt[1][0], nxt[2][1], nxt[3])
                self.attn_tiles(h, 1, True, Kn, None, Vv, Qn, None, pT, OC[h][1])
            if h % 2 == 1:
                self.conv_chunk(l, 4 + h // 2, ZC)
            cur = nxt
        self.conv_finish(l, ZC)

        k.set_rr(6)
        VB = l * VL["_per_layer"]
        wA, wB, wC, wO = self.W("mla_w_o", l), self.W("conv_w_pw", l), self.W("moba_w_o", l), self.W("w_out", l)
        gt = self.S("gt", l, "r")
        merged = [self.pool[16 + c] for c in range(16)]
        for t in range(NT):
            for c in range(NCH):
                ra = self.wst.next([self.wload(wA, 0, 1024, c * 128, 128, 0), self.wload(wB, 0, 1024, c * 128, 128, 1024),
                                    self.wload(wC, 0, 1024, c * 128, 128, 2048)],
                                   [(0, 8, 128, 128), (1024, 8, 128, 128), (2048, 8, 128, 128)])
                rb = self.wst.next([LD(gt.v(gt.ap[b, c * 128:(c + 1) * 128, t * TT:(t + 1) * TT].rearrange("(o p) n -> p o n", o=1),
                                            (b, c, t)), b * TT, 1, TT, q=SPq) for b in range(3)],
                                   [(b * TT, 1, TT, 128) for b in range(3)], cont=True)
                if k.planning:
                    continue
                srcs = (OA, ZC, OC)
                ys = []
                for b in range(3):
                    ps = k.ps()
                    k.mm_group(ps[:], [(TV(ra[b].ap[:, i, :], ra[b].bufs), srcs[b][i][t][:]) for i in range(8)])
                    ys.append(ps)
                m1, m2 = self.tmp(), self.tmp()
                k.tt(m1[:], ys[0][:], TV(rb[0].ap[:, 0, :], rb[0].bufs), ALU.mult)
                k.tt(m2[:], ys[1][:], TV(rb[1].ap[:, 0, :], rb[1].bufs), ALU.mult)
                k.tt(m1[:], m1[:], m2[:], ALU.add)
                k.tt(m2[:], ys[2][:], TV(rb[2].ap[:, 0, :], rb[2].bufs), ALU.mult)
                k.tt(merged[c][:], m1[:], m2[:], ALU.add)
            for d in range(NCH):
                res = self.wst.next([self.wload(wO, 0, D, d * 128, 128, 0)], [(0, NCH, 128, 128)])
                if k.planning:
                    continue
                (wo,) = res
                ps = k.ps()
                k.mm_group(ps[:], [(TV(wo.ap[:, c, :], wo.bufs), merged[c][:]) for c in range(NCH)])
                x = self.XT[d][t]
                k.tt(x[:], x[:], ps[:], ALU.add)

    def seg_A(self, l):
        self.ffn(l, "ffn1")
        self.mixer_in(l)

    def seg_B(self, l):
        self.mixer_out(l)
        self.ffn(l, "ffn2")


def build_program(segs, first, last, fused=False):
    nc = bass.Bass("TRN2", target_bir_lowering=False)
    es = ExitStack()
    with es:
        k = KB(nc, es)
        p = Prog(k, fused)

        def body():
            for (kind, l) in segs:
                if kind == "A":
                    p.seg_A(l)
                elif kind == "M1":
                    p.mixer_in(l)
                elif kind == "M2":
                    p.mixer_out(l)
                else:
                    p.seg_B(l)
        k.planning = True
        body()
        k.planning = False
        p.setup()
        p.load_x()
        body()
        p.store_x()
        p.finish()
        info = (list(p.in_names), list(p.out_names), dict(k.stats))
    return nc, info


def build_fused(depth=DEPTH):
    nc = bass.Bass("TRN2", target_bir_lowering=False)
    es = ExitStack()
    with es:
        k = KB(nc, es)
        p = Prog(k, True)

        def body():
            for l in range(depth):
                for v in range(2):
                    p.set_half(v)
                    p.load_xs(l == 0)
                    p.seg_A(l)
                    p.store_xs(False)
                for v in range(2):
                    p.set_half(v)
                    p.load_xs(False)
                    p.seg_B(l)
                    p.store_xs(l == depth - 1)
        k.planning = True
        body()
        k.planning = False
        p.setup()
        body()
        p.finish()
        info = (list(p.in_names), list(p.out_names), dict(k.stats))
    return nc, info


def pack_vecs(inputs):
    per = VL["_per_layer"]
    v = np.zeros((128, NVEC), np.float32)

    def put(l, name, arr):
        arr = np.asarray(arr, np.float32)
        v[:arr.shape[0], l * per + VL[name]: l * per + VL[name] + arr.shape[1]] = arr

    for l in range(DEPTH):
        for name in ("ffn1_norm", "mix_norm", "ffn2_norm"):
            put(l, name, np.asarray(inputs[name][l]).reshape(16, 128).T)
        put(l, "b_gate", np.asarray(inputs["b_gate"][l]).reshape(48, 128).T)
        put(l, "cq_norm", np.asarray(inputs["mla_cq_norm"][l]).reshape(6, 128).T)
        put(l, "ckv_norm", np.asarray(inputs["mla_ckv_norm"][l]).reshape(4, 128).T)
        for pre, src in (("qn", "mla_q_norm"), ("kn", "mla_k_norm")):
            g = np.asarray(inputs[src][l], np.float32)
            put(l, pre + "_nope", g[:128].reshape(128, 1))
            put(l, pre + "_rope", g[128:192].reshape(64, 1))
            put(l, pre + "_rperm", np.concatenate([g[160:192], g[128:160]]).reshape(64, 1))
        w = np.asarray(inputs["conv_w_dw"][l], np.float32)
        put(l, "conv_w", w.T.reshape(8, 128, 31).transpose(1, 0, 2).reshape(128, 248))
        put(l, "conv_b", np.asarray(inputs["conv_b_dw"][l]).reshape(8, 128).T)
        put(l, "ln_g", np.asarray(inputs["conv_ln_g"][l]).reshape(8, 128).T)
        put(l, "ln_b", np.asarray(inputs["conv_ln_b"][l]).reshape(8, 128).T)
        put(l, "mq_norm", np.asarray(inputs["moba_q_norm"][l]).reshape(128, 1))
        put(l, "mk_norm", np.asarray(inputs["moba_k_norm"][l]).reshape(128, 1))
    return v


def make_consts(half):
    import ml_dtypes
    pos = (half * T + np.arange(T)).astype(np.float32)
    inv = np.exp(-np.log(10000.0) * np.arange(32, dtype=np.float32) * 2.0 / 64.0).astype(np.float32)
    ang = (pos[None, :] * inv[:, None]).astype(np.float32)
    cos, sin = np.cos(ang), np.sin(ang)
    rope = np.zeros((64, 2 * T), np.float32)
    rope[0:32, 0:T] = cos
    rope[32:64, 0:T] = cos
    rope[0:32, T:] = -sin
    rope[32:64, T:] = sin
    key = np.arange(128, dtype=np.float32)[:, None]
    j = np.arange(896, dtype=np.float32)[None, :] - 384.0
    mask = np.zeros((128, CB_COLS), np.float32)
    mask[:, 0:896] = np.where(key <= j, 0.0, NEG)
    mask[:, 896:1792] = np.where(key <= j, key - j, -1.0e6)
    mask[:, 1792:2304] = key - np.arange(512, dtype=np.float32)[None, :]
    cs = np.zeros((128, CS_COLS), np.float32)
    cs[:, 0] = 0.0 if half == 1 else NEG
    cs[:, 1] = 1.0 if half == 1 else 0.0
    for s in range(8):
        qbl = s // 2
        for slot in range(8):
            if slot < 4:
                past, own = (half == 1), False
            else:
                past, own = (slot - 4 < qbl), (slot - 4 == qbl)
            cs[:, 2 + s * 8 + slot] = 0.0 if past else -1.0e30
            cs[:, 66 + s * 8 + slot] = 0.0 if (past or own) else NEG
            cs[:, 130 + s * 8 + slot] = 0.0 if own else 1.0
    cbf = np.zeros((128, CBF_COLS), np.float32)
    cbf[:, 0:128] = np.eye(128, dtype=np.float32)
    for slot in range(8):
        cbf[slot, 128 + slot * 128:128 + (slot + 1) * 128] = 1.0
    return {"rope_in": rope, "mask_in": mask, "cs_in": cs, "cbf_in": cbf.astype(ml_dtypes.bfloat16)}


N_CORES = 4


def kernel(**inputs):
    x = np.asarray(inputs["x"], np.float32)
    vecs = pack_vecs(inputs)
    c0, c1 = make_consts(0), make_consts(1)
    nc, (in_names, out_names, stats) = build_fused()
    shared = {"vecs_in": vecs, "cbf_in": c0["cbf_in"], "mask_in": c0["mask_in"],
              "rope_in": np.stack([c0["rope_in"], c1["rope_in"]]), "cs_in": np.stack([c0["cs_in"], c1["cs_in"]])}
    for name in in_names:
        if name not in shared and name != "xT":
            base, l = name.rsplit("_", 1)
            shared[name] = np.ascontiguousarray(np.asarray(inputs[base][int(l)], np.float32))
    in_maps = []
    for b in range(N_CORES):
        m = {name: shared[name] for name in in_names if name != "xT"}
        m["xT"] = np.ascontiguousarray(x[b].reshape(2, T, D).transpose(0, 2, 1))
        in_maps.append(m)
    res = run_bass_kernel_spmd(nc, in_maps, core_ids=list(range(N_CORES)))
    out = np.empty((NB, SEQ, D), np.float32)
    for b in range(N_CORES):
        out[b] = res.results[b]["yT"].transpose(0, 2, 1).reshape(SEQ, D)
    return out
```

```python
import numpy as np
from contextlib import ExitStack
import concourse.bass as bass
import concourse.mybir as mybir
from concourse.bass_utils import run_bass_kernel_spmd

F32 = mybir.dt.float32
BF16 = mybir.dt.bfloat16
AF = mybir.ActivationFunctionType
ALU = mybir.AluOpType
AX = mybir.AxisListType

D = 2048
SEQ = 2048
NB = 4
DEPTH = 2
DFF = 5632
NCH = D // 128
T = 1024
TT = 512
NT = T // TT
FCH = DFF // 128
EPS = 1e-6

SAME_ENGINE_SYNC = True


class Buf:
    __slots__ = ("name", "writer", "readers", "excl")

    def __init__(self, name):
        self.name = name
        self.writer = None
        self.readers = []
        self.excl = False


class TV:
    __slots__ = ("ap", "bufs", "tile")

    def __init__(self, ap, bufs, tile=None):
        self.ap = ap
        self.bufs = bufs
        self.tile = tile


class Tile:
    def __init__(self, k, name, shape, dtype, space="sbuf"):
        self.k = k
        self.name = name
        self.shape = shape
        if space == "sbuf":
            self.t = k.es.enter_context(k.nc.sbuf_tensor(name, shape, dtype))
        else:
            self.t = k.es.enter_context(k.nc.psum_tensor(name, shape, dtype))
        self.buf = Buf(name)
        self.buf.excl = (space != "sbuf")
        self.dsem = None
        self.dcount = 0

    def __getitem__(self, idx):
        return TV(self.t[idx], [self.buf], self)

    def view(self, fn):
        return TV(fn(self.t), [self.buf], self)


class DTile:
    def __init__(self, k, name, shape, dtype, kind):
        self.k = k
        self.name = name
        self.h = k.nc.dram_tensor(name, shape, dtype, kind=kind)
        self.ap = self.h.ap()
        self.bufs = {}

    def v(self, ap, *keys):
        bl = []
        for key in keys:
            if key not in self.bufs:
                self.bufs[key] = Buf(f"{self.name}:{key}")
            bl.append(self.bufs[key])
        return TV(ap, bl, None)


class Eng:
    def __init__(self, k, name, eng):
        self.name = name
        self.eng = eng
        self.sem = k.es.enter_context(k.nc.semaphore("s_" + name))
        self.n = 0
        self.seen = {}


class KB:
    def __init__(self, nc, es):
        self.nc = nc
        self.es = es
        self.PE = Eng(self, "pe", nc.tensor)
        self.ACT = Eng(self, "act", nc.scalar)
        self.DVE = Eng(self, "dve", nc.vector)
        self.POOL = Eng(self, "pool", nc.gpsimd)
        self.SP = Eng(self, "sp", nc.sync)
        self.sems = {}
        for e in (self.PE, self.ACT, self.DVE, self.POOL, self.SP):
            self.sems[id(e.sem)] = e.sem
        self.planning = False
        self.nsem = 5
        self.psum = [Tile(self, f"ps{i}", [128, 512], F32, "psum") for i in range(8)]
        self.psi = 0
        self.rr = 8
        self.stats = {"waits": 0, "ops": 0, "dmas": 0}

    def new_dsem(self, name):
        s = self.es.enter_context(self.nc.semaphore("d_" + name))
        self.sems[id(s)] = s
        self.nsem += 1
        return [s, 0]

    def set_rr(self, n):
        self.rr = n
        self.psi = 0

    def ps(self):
        p = self.psum[self.psi]
        self.psi = (self.psi + 1) % self.rr
        return p

    def _sync(self, E, reads, writes):
        deps = {}
        for tv in reads:
            for b in tv.bufs:
                if b.writer is not None:
                    s, v = b.writer
                    deps[s] = max(deps.get(s, 0), v)
                if b.excl:
                    for (s, v) in b.readers:
                        if s != id(E.sem):
                            deps[s] = max(deps.get(s, 0), v)
        for tv in writes:
            for b in tv.bufs:
                if b.writer is not None:
                    s, v = b.writer
                    deps[s] = max(deps.get(s, 0), v)
                for (s, v) in b.readers:
                    deps[s] = max(deps.get(s, 0), v)
        for s, v in deps.items():
            if s == id(E.sem) and (E is self.PE or not SAME_ENGINE_SYNC):
                continue
            if E.seen.get(s, 0) < v:
                E.eng.wait_ge(self.sems[s], v)
                E.seen[s] = v
                self.stats["waits"] += 1

    def _mark(self, key, val, reads, writes):
        for tv in reads:
            for b in tv.bufs:
                b.readers.append((key, val))
        for tv in writes:
            for b in tv.bufs:
                b.writer = (key, val)
                b.readers = []

    def op(self, E, fn, reads, writes):
        if self.planning:
            return
        self._sync(E, reads, writes)
        ins = fn()
        E.n += 1
        ins.then_inc(E.sem, 1)
        self._mark(id(E.sem), E.n, reads, writes)
        self.stats["ops"] += 1

    def dma(self, Q, out, in_, dsem):
        if self.planning:
            return
        self._sync(Q, [in_], [out])
        ins = Q.eng.dma_start(out=out.ap, in_=in_.ap)
        dsem[1] += 16
        ins.then_inc(dsem[0], 16)
        self._mark(id(dsem[0]), dsem[1], [in_], [out])
        self.stats["dmas"] += 1

    def wait_all(self, E, tvs):
        if self.planning:
            return
        self._sync(E, tvs, [])

    def mm_group(self, out, pairs):
        reads = []
        for a, b in pairs:
            reads.append(a)
            reads.append(b)
        n = len(pairs)

        def fn():
            ins = None
            for i, (a, b) in enumerate(pairs):
                ins = self.nc.tensor.matmul(out.ap, lhsT=a.ap, rhs=b.ap, start=(i == 0), stop=(i == n - 1))
            return ins
        self.op(self.PE, fn, reads, [out])

    def act(self, out, in_, func, bias=None, scale=None, extra_reads=()):
        kw = {}
        if bias is not None:
            kw["bias"] = bias.ap if isinstance(bias, TV) else bias
        if scale is not None:
            kw["scale"] = scale.ap if isinstance(scale, TV) else scale
        reads = [in_] + [x for x in (bias, scale) if isinstance(x, TV)] + list(extra_reads)
        self.op(self.ACT, lambda: self.nc.scalar.activation(out=out.ap, in_=in_.ap, func=func, **kw), reads, [out])

    def tt(self, out, in0, in1, op, E=None):
        E = E or self.DVE
        self.op(E, lambda: E.eng.tensor_tensor(out=out.ap, in0=in0.ap, in1=in1.ap, op=op), [in0, in1], [out])

    def ts(self, out, in0, s1, s2, op0, op1=None, E=None):
        E = E or self.DVE
        reads = [in0] + [x for x in (s1, s2) if isinstance(x, TV)]
        a1 = s1.ap if isinstance(s1, TV) else s1
        a2 = s2.ap if isinstance(s2, TV) else s2
        if op1 is None:
            fn = lambda: E.eng.tensor_scalar(out=out.ap, in0=in0.ap, scalar1=a1, scalar2=None, op0=op0)
        else:
            fn = lambda: E.eng.tensor_scalar(out=out.ap, in0=in0.ap, scalar1=a1, scalar2=a2, op0=op0, op1=op1)
        self.op(E, fn, reads, [out])

    def stt(self, out, in0, scalar, in1, op0, op1, E=None):
        E = E or self.DVE
        reads = [in0, in1] + ([scalar] if isinstance(scalar, TV) else [])
        sc = scalar.ap if isinstance(scalar, TV) else scalar
        self.op(E, lambda: E.eng.scalar_tensor_tensor(out=out.ap, in0=in0.ap, scalar=sc, in1=in1.ap, op0=op0, op1=op1),
                reads, [out])

    def copy(self, out, in_, E=None):
        E = E or self.DVE
        self.op(E, lambda: E.eng.tensor_copy(out=out.ap, in_=in_.ap), [in_], [out])

    def recip(self, out, in_):
        self.op(self.DVE, lambda: self.nc.vector.reciprocal(out=out.ap, in_=in_.ap), [in_], [out])

    def memset(self, out, val, E=None):
        E = E or self.DVE
        self.op(E, lambda: E.eng.memset(out.ap, val), [], [out])


def LD(src, off, K, N, c0=0, n=None, P=128, q=None):
    return dict(src=src, off=off, K=K, N=N, c0=c0, n=(N if n is None else n), P=P, q=q)


class Stream:
    def __init__(self, k, name, nslots, slot_elems):
        self.k = k
        self.nslots = nslots
        self.slots = [Tile(k, f"{name}{i}", [128, slot_elems], BF16) for i in range(nslots)]
        self.dsems = [k.new_dsem(f"{name}{i}") for i in range(nslots)]
        self.dsems_hw = [k.new_dsem(f"{name}h{i}") for i in range(nslots)]
        self.slot_elems = slot_elems
        self.plan = []
        self.issued = 0
        self.consumed = 0

    def _view(self, tile, off, K, N, P=128):
        assert off + K * N <= self.slot_elems
        return TV(tile.t[0:P, off:off + K * N].rearrange("p (k n) -> p k n", k=K), [tile.buf], tile)

    def _issue(self, i):
        loads, views = self.plan[i]
        s = i % self.nslots
        tile = self.slots[s]
        for ld in loads:
            v = self._view(tile, ld["off"], ld["K"], ld["N"], ld["P"])
            dst = TV(v.ap[:, :, ld["c0"]:ld["c0"] + ld["n"]], [tile.buf], tile)
            sem = self.dsems[s] if ld["q"] is self.k.POOL else self.dsems_hw[s]
            self.k.dma(ld["q"], dst, ld["src"], sem)

    def idx(self):
        return len(self.plan) if self.k.planning else self.consumed

    def next(self, loads, views, cont=False, anchor=None):
        if self.k.planning:
            self.plan.append((loads, views))
            return [None for _ in views]
        i = self.consumed
        if anchor is not None:
            self.gfirst = anchor
        elif not cont:
            self.gfirst = i
        while self.issued < min(len(self.plan), self.gfirst + self.nslots):
            self._issue(self.issued)
            self.issued += 1
        self.consumed += 1
        tile = self.slots[i % self.nslots]
        return [self._view(tile, *v) for v in self.plan[i][1]]


CQ0, CKV0, KR0, CONV0, MQ0, MK0, MV0, G0 = 0, 768, 1280, 1344, 3392, 4416, 5440, 6464
DIN = 12608
NEG = -30000.0
STOP = None
MLA_SCALE = 192.0 ** -0.5
MOBA_SCALE = 128.0 ** -0.5

WSHAPES = {
    "ffn1_w_gate": [D, DFF], "ffn1_w_up": [D, DFF], "ffn1_w_down": [DFF, D],
    "ffn2_w_gate": [D, DFF], "ffn2_w_up": [D, DFF], "ffn2_w_down": [DFF, D],
    "w_in": [D, DIN], "mla_w_uq": [768, 1536], "mla_w_ukv": [512, 2048],
    "mla_w_o": [1024, D], "conv_w_pw": [1024, D], "moba_w_o": [1024, D], "w_out": [D, D],
}
SSHAPES = {
    "qa": ([8, 192, T], BF16), "ka": ([8, 192, T], BF16), "va": ([T, 1024], BF16),
    "qc": ([8, 128, T], BF16), "kc": ([8, 128, T], BF16), "vc": ([T, 1024], BF16),
    "zc": ([1024, T], F32), "gt": ([3, D, T], BF16),
}
REMOTE = ("ka", "va", "kc", "vc", "zc")


def vec_layout():
    off = {}
    o = 0
    for name, n in (("ffn1_norm", 16), ("mix_norm", 16), ("ffn2_norm", 16), ("b_gate", 48), ("cq_norm", 6),
                    ("ckv_norm", 4), ("qn_nope", 1), ("qn_rope", 1), ("qn_rperm", 1), ("kn_nope", 1),
                    ("kn_rope", 1), ("kn_rperm", 1), ("conv_w", 248), ("conv_b", 8), ("ln_g", 8), ("ln_b", 8),
                    ("mq_norm", 1), ("mk_norm", 1)):
        off[name] = o
        o += n
    off["_per_layer"] = o
    return off


VL = vec_layout()
NVEC = VL["_per_layer"] * DEPTH
CB_COLS = 2304
CS_COLS = 2 + 192
CBF_COLS = 128 + 1024


class Prog:
    def __init__(self, k, fused):
        self.k = k
        self.fused = fused
        self.dts = {}
        self.in_names = []
        self.out_names = []
        self.XT = [[Tile(k, f"xt{c}_{t}", [128, TT], F32) for t in range(NT)] for c in range(NCH)]
        self.HT = [[Tile(k, f"ht{c}_{t}", [128, TT], BF16) for t in range(NT)] for c in range(NCH)]
        self.pool = [Tile(k, f"pl{i}", [128, TT], BF16) for i in range(32)]
        self.ones = Tile(k, "ones", [128, 128], BF16)
        self.sq = [Tile(k, f"sq{i}", [128, TT], BF16) for i in range(2)]
        self.rstd = [Tile(k, f"rstd{i}", [128, TT], F32) for i in range(2)]
        self.tmpf = [Tile(k, f"tmpf{i}", [128, TT], F32) for i in range(4)]
        self.tmpi = 0
        self.stg = [Tile(k, f"stg{i}", [128, TT], BF16) for i in range(4)]
        self.stgsem = [k.new_dsem(f"stg{i}") for i in range(4)]
        self.stgi = 0
        self.tmpsem = [k.new_dsem(f"tmpf{i}") for i in range(4)]
        self.vecs = Tile(k, "vecs", [128, NVEC], F32)
        self.cbuf = Tile(k, "cbuf", [128, CB_COLS], F32)
        self.cbsem = k.new_dsem("cbuf")
        self.cs = Tile(k, "cs", [128, CS_COLS], F32)
        self.cbf = Tile(k, "cbf", [128, CBF_COLS], BF16)
        self.kr = Tile(k, "kr", [64, T], F32)
        self.ssr = Tile(k, "ssr", [128, T], F32)
        self.zb = self.ssr
        self.zbsem = k.new_dsem("zb")
        self.km = Tile(k, "km", [128, 16], F32)
        self.kmb = Tile(k, "kmb", [128, 32], BF16)
        self.gm = Tile(k, "gm", [128, 64], F32)
        self.cnt = Tile(k, "cnt", [128, 64], F32)
        self.cmp = Tile(k, "cmp", [128, 64], F32)
        self.pen = Tile(k, "pen", [128, 64], F32)
        self.penb = Tile(k, "penb", [128, 64], BF16)
        self.penT = [Tile(k, f"penT{i}", [8, T], BF16) for i in range(2)]
        self.wst = Stream(k, "w", 4, 4096)
        self.xsem = k.new_dsem("x")
        self.xssem = k.new_dsem("xs")
        self.csem = k.new_dsem("c")
        self.cssem = k.new_dsem("cs")
        self.v = 0

    def dram(self, name, shape, dtype, kind):
        if name not in self.dts:
            self.dts[name] = DTile(self.k, name, shape, dtype, kind)
            if kind == "ExternalInput":
                self.in_names.append(name)
            elif kind == "ExternalOutput":
                self.out_names.append(name)
        return self.dts[name]

    def W(self, name, l):
        return self.dram(f"{name}_{l}", WSHAPES[name], F32, "ExternalInput")

    def S(self, name, l, mode):
        shape, dt = SSHAPES[name]
        if self.fused:
            vv = 0 if mode == "rr" else self.v
            return self.dram(f"{name}_{l}_{vv}", shape, dt, "Internal")
        if mode == "w":
            return self.dram(f"{name}_{l}", shape, dt, "ExternalOutput")
        if mode == "r":
            return self.dram(f"{name}_{l}_own", shape, dt, "ExternalInput")
        return self.dram(f"{name}_{l}_rem", shape, dt, "ExternalInput")

    def tmp(self):
        i = self.tmpi
        self.tmpi = (self.tmpi + 1) % len(self.tmpf)
        return self.tmpf[i]

    def tmp_i(self):
        i = self.tmpi
        self.tmpi = (self.tmpi + 1) % len(self.tmpf)
        return i

    def stage(self):
        i = self.stgi
        self.stgi = (self.stgi + 1) % len(self.stg)
        return i

    def wload(self, w, r0, nrows, c0, ncols, off, c0d=0, Ntot=None):
        src = w.v(w.ap[r0:r0 + nrows, c0:c0 + ncols].rearrange("(k p) n -> p k n", p=128), "all")
        return LD(src, off, nrows // 128, Ntot or ncols, c0d, ncols, 128, self.k.POOL)

    def setup(self):
        k = self.k
        k.memset(self.ones[:], 1.0)
        vin = self.dram("vecs_in", [128, NVEC], F32, "ExternalInput")
        cbfin = self.dram("cbf_in", [128, CBF_COLS], BF16, "ExternalInput")
        k.dma(k.SP, self.vecs[:], vin.v(vin.ap, "all"), self.csem)
        k.dma(k.SP, self.cbf[:], cbfin.v(cbfin.ap, "all"), self.csem)
        if not self.fused:
            csin = self.dram("cs_in", [128, CS_COLS], F32, "ExternalInput")
            k.dma(k.SP, self.cs[:], csin.v(csin.ap, "all"), self.csem)
        if not k.planning:
            tot = (id(self.csem[0]), self.csem[1])
            for tl in (self.vecs, self.cbf) + (() if self.fused else (self.cs,)):
                tl.buf.writer = tot

    def set_half(self, v):
        self.v = v
        csin = self.dram("cs_in", [2, 128, CS_COLS], F32, "ExternalInput")
        self.k.dma(self.k.SP, self.cs[:], csin.v(csin.ap[v], "all"), self.cssem)

    def load_cbuf(self, which):
        k = self.k
        if which == "rope":
            if self.fused:
                src = self.dram("rope_in", [2, 64, 2 * T], F32, "ExternalInput")
                k.dma(k.SP, self.cbuf[0:64, 0:2 * T], src.v(src.ap[self.v], "all"), self.cbsem)
            else:
                src = self.dram("rope_in", [64, 2 * T], F32, "ExternalInput")
                k.dma(k.SP, self.cbuf[0:64, 0:2 * T], src.v(src.ap, "all"), self.cbsem)
        else:
            src = self.dram("mask_in", [128, CB_COLS], F32, "ExternalInput")
            k.dma(k.SP, self.cbuf[:], src.v(src.ap, "all"), self.cbsem)

    def load_x(self):
        k = self.k
        xT = self.dram("xT", [D, T], F32, "ExternalInput")
        for c in range(NCH):
            for t in range(NT):
                k.dma(k.SP, self.XT[c][t][:], xT.v(xT.ap[c * 128:(c + 1) * 128, t * TT:(t + 1) * TT], "all"), self.xsem)
        if not k.planning:
            tot = (id(self.xsem[0]), self.xsem[1])
            for c in range(NCH):
                for t in range(NT):
                    self.XT[c][t].buf.writer = tot

    def load_xs(self, first):
        k = self.k
        src = self.dram("xT", [2, D, T], F32, "ExternalInput") if first else self.dram("xs", [2, D, T], F32, "Internal")
        v = self.v
        for c in range(NCH):
            for t in range(NT):
                k.dma(k.SP, self.XT[c][t][:], src.v(src.ap[v, c * 128:(c + 1) * 128, t * TT:(t + 1) * TT], (v, c, t)), self.xsem)
        if not k.planning:
            sid = id(self.xsem[0])
            tot = (sid, self.xsem[1])
            for c in range(NCH):
                for t in range(NT):
                    self.XT[c][t].buf.writer = tot
                    b = src.bufs[(v, c, t)]
                    b.readers = [(s_, v_) if s_ != sid else tot for (s_, v_) in b.readers]

    def store_xs(self, last):
        k = self.k
        dst = self.dram("yT", [2, D, T], F32, "ExternalOutput") if last else self.dram("xs", [2, D, T], F32, "Internal")
        v = self.v
        for c in range(NCH):
            for t in range(NT):
                k.dma(k.SP, dst.v(dst.ap[v, c * 128:(c + 1) * 128, t * TT:(t + 1) * TT], (v, c, t)), self.XT[c][t][:], self.xssem)
        if not k.planning:
            sid = id(self.xssem[0])
            tot = (sid, self.xssem[1])
            for c in range(NCH):
                for t in range(NT):
                    b = self.XT[c][t].buf
                    b.readers = [(s_, v_) if s_ != sid else tot for (s_, v_) in b.readers]
                    dst.bufs[(v, c, t)].writer = tot

    def store_x(self):
        k = self.k
        yT = self.dram("yT", [D, T], F32, "ExternalOutput")
        for c in range(NCH):
            for t in range(NT):
                k.dma(k.SP, yT.v(yT.ap[c * 128:(c + 1) * 128, t * TT:(t + 1) * TT], (c, t)), self.XT[c][t][:], self.xsem)

    def finish(self):
        k = self.k
        if k.planning:
            return
        for (sem, cnt) in [self.xsem, self.xssem] + self.stgsem + self.tmpsem:
            if cnt > 0:
                k.SP.eng.wait_ge(sem, cnt)

    def rstd_from(self, ss, dim, out, P=128):
        k = self.k
        tm = self.tmp()
        k.act(tm[0:P, :], ss, AF.Sqrt, bias=EPS, scale=1.0 / dim)
        k.recip(out, tm[0:P, :])

    def ones_mm(self, ps, pairs):
        k = self.k
        k.mm_group(ps, [(self.ones[0:P, :], src) for (src, P) in pairs])

    def store(self, dst, si, view=None):
        k = self.k
        src = self.stg[si][:] if view is None else view
        k.dma(k.SP, dst, src, self.stgsem[si])

    def rmsnorm_x(self, gcol):
        k = self.k
        k.set_rr(6)
        for t in range(NT):
            ps = k.psum[6 + t]
            for c in range(NCH):
                sq = self.sq[c % 2]
                k.act(sq[:], self.XT[c][t][:], AF.Square)
                k.op(k.PE, (lambda c=c, sq=sq, ps=ps: k.nc.tensor.matmul(ps.t[:], lhsT=self.ones.t[:], rhs=sq.t[:],
                                                                          start=(c == 0), stop=(c == NCH - 1))),
                     [self.ones[:], sq[:]], [ps[:]])
            r = self.rstd[t]
            self.rstd_from(ps[:], D, r[:])
            for c in range(NCH):
                k.stt(self.HT[c][t][:], self.XT[c][t][:], self.vecs[:, gcol + c:gcol + c + 1], r[:], ALU.mult, ALU.mult)

    def mm2(self, pss, wt, kchunks, cols, rhs):
        k = self.k
        reads = [wt] + [rhs[c][t][:] for c in range(kchunks) for t in range(NT)]

        def fn():
            ins = None
            for c in range(kchunks):
                for t in range(NT):
                    ins = k.nc.tensor.matmul(pss[t].ap, lhsT=wt.ap[:, c, cols[0]:cols[1]], rhs=rhs[c][t].t[:],
                                             start=(c == 0), stop=(c == kchunks - 1))
            return ins
        k.op(k.PE, fn, reads, list(pss))

    def ffn(self, l, which):
        k = self.k
        wg, wu, wd = self.W(which + "_w_gate", l), self.W(which + "_w_up", l), self.W(which + "_w_down", l)
        gcol = l * VL["_per_layer"] + VL[which + "_norm"]
        self.rmsnorm_x(gcol)
        k.set_rr(8)
        actT = [[self.pool[j * NT + t] for t in range(NT)] for j in range(11)]
        QCH = FCH // 4
        for q in range(4):
            for jj in range(QCH):
                j = q * QCH + jj
                res = self.wst.next([self.wload(wg, 0, D, j * 128, 128, 0), self.wload(wu, 0, D, j * 128, 128, 2048)],
                                    [(0, NCH, 128, 128), (2048, NCH, 128, 128)])
                if k.planning:
                    continue
                wgt, wut = res
                psg = [k.ps() for _ in range(NT)]
                psu = [k.ps() for _ in range(NT)]
                self.mm2([p[:] for p in psg], wgt, NCH, (0, 128), self.HT)
                self.mm2([p[:] for p in psu], wut, NCH, (0, 128), self.HT)
                for t in range(NT):
                    tm = self.tmp()
                    k.act(tm[:], psg[t][:], AF.Silu)
                    k.tt(actT[jj][t][:], tm[:], psu[t][:], ALU.mult)
            for db in range(D // 256):
                res = self.wst.next([self.wload(wd, q * QCH * 128, QCH * 128, db * 256, 256, 0)], [(0, QCH, 256, 128)])
                if k.planning:
                    continue
                (wdt,) = res
                for dd in range(2):
                    d = db * 2 + dd
                    pss = [k.ps() for _ in range(NT)]
                    self.mm2([p[:] for p in pss], wdt, QCH, (dd * 128, dd * 128 + 128), actT)
                    for t in range(NT):
                        x = self.XT[d][t]
                        k.stt(x[:], pss[t][:], 0.5, x[:], ALU.mult, ALU.add)

    def mixer_in(self, l):
        k = self.k
        VB = l * VL["_per_layer"]
        vc = lambda name, i=0, P=128: self.vecs[0:P, VB + VL[name] + i:VB + VL[name] + i + 1]
        self.rmsnorm_x(VB + VL["mix_norm"])
        k.set_rr(6)
        self.load_cbuf("rope")
        if STOP == 'setup':
            return
        w_in = self.W("w_in", l)
        w_uq = self.W("mla_w_uq", l)
        w_ukv = self.W("mla_w_ukv", l)
        qa, ka, va = self.S("qa", l, "w"), self.S("ka", l, "w"), self.S("va", l, "w")
        qc, kc, vcd = self.S("qc", l, "w"), self.S("kc", l, "w"), self.S("vc", l, "w")
        zc, gt = self.S("zc", l, "w"), self.S("gt", l, "w")
        cq = [[self.pool[i * NT + t] for t in range(NT)] for i in range(6)]
        ckv = [[self.pool[12 + i * NT + t] for t in range(NT)] for i in range(4)]
        tsl = lambda t: slice(t * TT, (t + 1) * TT)

        def latent(col0, nch, dst, gname, dim):
            ss = [k.psum[6], k.psum[7]]
            for b in range(0, nch, 2):
                res = self.wst.next([self.wload(w_in, 0, D, col0 + b * 128, 256, 0)], [(0, NCH, 256, 128)])
                if k.planning:
                    continue
                (wt,) = res
                for i in range(2):
                    ci = b + i
                    pss = [k.ps() for _ in range(NT)]
                    self.mm2([p[:] for p in pss], wt, NCH, (i * 128, i * 128 + 128), self.HT)
                    for t in range(NT):
                        sq = self.sq[t]
                        k.act(sq[:], pss[t][:], AF.Square)
                        k.copy(dst[ci][t][:], pss[t][:])
                        k.op(k.PE, (lambda t=t, sq=sq, ci=ci: k.nc.tensor.matmul(
                            ss[t].t[:], lhsT=self.ones.t[:], rhs=sq.t[:], start=(ci == 0), stop=(ci == nch - 1))),
                            [self.ones[:], sq[:]], [ss[t][:]])
            if k.planning:
                return
            for t in range(NT):
                self.rstd_from(ss[t][:], dim, self.rstd[t][:])
                for i in range(nch):
                    k.stt(dst[i][t][:], dst[i][t][:], vc(gname, i), self.rstd[t][:], ALU.mult, ALU.mult)

        latent(CQ0, 6, cq, "cq_norm", 768.0)
        latent(CKV0, 4, ckv, "ckv_norm", 512.0)
        if STOP == 'latent':
            return

        res = self.wst.next([self.wload(w_in, 0, D, KR0, 64, 0),
                             self.wload(w_in, 0, D, KR0 + 32, 32, 1024, 0, 64),
                             self.wload(w_in, 0, D, KR0, 32, 1024, 32, 64)],
                            [(0, NCH, 64, 128), (1024, NCH, 64, 128)])
        if not k.planning:
            wa, wb = res
            psa = [k.ps() for _ in range(NT)]
            psb = [k.ps() for _ in range(NT)]
            self.mm2([p[0:64, :] for p in psa], wa, NCH, (0, 64), self.HT)
            self.mm2([p[0:64, :] for p in psb], wb, NCH, (0, 64), self.HT)
            for t in range(NT):
                sq = self.sq[t]
                k.act(sq[0:64, :], psa[t][0:64, :], AF.Square)
                pss = k.ps()
                self.ones_mm(pss[:], [(sq[0:64, :], 64)])
                k.copy(self.ssr[:, tsl(t)], pss[:])
                t1, t2 = self.tmp(), self.tmp()
                k.stt(t1[0:64, :], psa[t][0:64, :], vc("kn_rope", 0, 64), self.cbuf[0:64, tsl(t)], ALU.mult, ALU.mult)
                k.stt(t2[0:64, :], psb[t][0:64, :], vc("kn_rperm", 0, 64), self.cbuf[0:64, T + t * TT:T + (t + 1) * TT],
                      ALU.mult, ALU.mult)
                k.tt(self.kr[0:64, tsl(t)], t1[0:64, :], t2[0:64, :], ALU.add)

        if STOP == 'kr':
            return
        for h in range(8):
            c0 = h * 192
            res = self.wst.next([self.wload(w_uq, 0, 768, c0, 128, 0),
                                 self.wload(w_uq, 0, 768, c0 + 128, 64, 768),
                                 self.wload(w_uq, 0, 768, c0 + 160, 32, 1152, 0, 64),
                                 self.wload(w_uq, 0, 768, c0 + 128, 32, 1152, 32, 64)],
                                [(0, 6, 128, 128), (768, 6, 64, 128), (1152, 6, 64, 128)])
            if k.planning:
                continue
            wn, wr, ws = res
            psn = [k.ps() for _ in range(NT)]
            self.mm2([p[:] for p in psn], wn, 6, (0, 128), cq)
            psr = [k.ps() for _ in range(NT)]
            self.mm2([p[0:64, :] for p in psr], wr, 6, (0, 64), cq)
            pss_ = [k.ps() for _ in range(NT)]
            self.mm2([p[0:64, :] for p in pss_], ws, 6, (0, 64), cq)
            for t in range(NT):
                k.act(self.sq[0][:], psn[t][:], AF.Square)
                k.act(self.sq[1][0:64, :], psr[t][0:64, :], AF.Square)
                pst = k.psum[6 + t]
                self.ones_mm(pst[:], [(self.sq[0][:], 128), (self.sq[1][0:64, :], 64)])
                r = self.tmp()
                self.rstd_from(pst[:], 192.0, r[:])
                si = self.stage()
                k.stt(self.stg[si][:], psn[t][:], vc("qn_nope"), r[:], ALU.mult, ALU.mult)
                self.store(qa.v(qa.ap[h, 0:128, tsl(t)], (h, t, 0)), si)
                t1, t2 = self.tmp(), self.tmp()
                k.stt(t1[0:64, :], psr[t][0:64, :], vc("qn_rope", 0, 64), self.cbuf[0:64, tsl(t)], ALU.mult, ALU.mult)
                k.stt(t2[0:64, :], pss_[t][0:64, :], vc("qn_rperm", 0, 64), self.cbuf[0:64, T + t * TT:T + (t + 1) * TT],
                      ALU.mult, ALU.mult)
                k.tt(t1[0:64, :], t1[0:64, :], t2[0:64, :], ALU.add)
                si = self.stage()
                k.tt(self.stg[si][0:64, :], t1[0:64, :], r[0:64, :], ALU.mult)
                self.store(qa.v(qa.ap[h, 128:192, tsl(t)], (h, t, 1)), si, self.stg[si][0:64, :])

        if STOP == 'q':
            return
        for h in range(8):
            res = self.wst.next([self.wload(w_ukv, 0, 512, h * 256, 128, 0)], [(0, 4, 128, 128)])
            if k.planning:
                continue
            (wk,) = res
            psk = [k.ps() for _ in range(NT)]
            self.mm2([p[:] for p in psk], wk, 4, (0, 128), ckv)
            for t in range(NT):
                k.act(self.sq[t][:], psk[t][:], AF.Square)
                pst = k.psum[6 + t]
                self.ones_mm(pst[:], [(self.sq[t][:], 128)])
                tot = self.tmp()
                k.tt(tot[:], pst[:], self.ssr[:, tsl(t)], ALU.add)
                r = self.tmp()
                self.rstd_from(tot[:], 192.0, r[:])
                si = self.stage()
                k.stt(self.stg[si][:], psk[t][:], vc("kn_nope"), r[:], ALU.mult, ALU.mult)
                self.store(ka.v(ka.ap[h, 0:128, tsl(t)], (h, t, 0)), si)
                si = self.stage()
                k.tt(self.stg[si][0:64, :], self.kr[0:64, tsl(t)], r[0:64, :], ALU.mult)
                self.store(ka.v(ka.ap[h, 128:192, tsl(t)], (h, t, 1)), si, self.stg[si][0:64, :])

        if STOP == 'k':
            return
        for g in range(2):
            res = self.wst.next([self.wload(w_ukv, 0, 512, (4 * g + hh) * 256 + 128, 128, 0, hh * 128, 512) for hh in range(4)],
                                [(0, 4, 512, 128)])
            if k.planning:
                continue
            (wv,) = res
            for t in range(NT):
                for sb in range(4):
                    ps = k.ps()
                    k.mm_group(ps[:], [(TV(ckv[i][t].t[:, sb * 128:(sb + 1) * 128], [ckv[i][t].buf]),
                                        TV(wv.ap[:, i, :], wv.bufs)) for i in range(4)])
                    si = self.stage()
                    k.copy(self.stg[si][:], ps[:])
                    r0 = t * TT + sb * 128
                    self.store(va.v(va.ap[r0:r0 + 128, g * 512:(g + 1) * 512], (r0, g)), si)

        if STOP == 'v':
            return
        for (col0, dst, gname) in ((MQ0, qc, "mq_norm"), (MK0, kc, "mk_norm")):
            for hb in range(4):
                res = self.wst.next([self.wload(w_in, 0, D, col0 + hb * 256, 256, 0)], [(0, NCH, 256, 128)])
                if k.planning:
                    continue
                (wt,) = res
                for i in range(2):
                    h = hb * 2 + i
                    pss = [k.ps() for _ in range(NT)]
                    self.mm2([p[:] for p in pss], wt, NCH, (i * 128, i * 128 + 128), self.HT)
                    for t in range(NT):
                        k.act(self.sq[t][:], pss[t][:], AF.Square)
                        pst = k.psum[6 + t]
                        self.ones_mm(pst[:], [(self.sq[t][:], 128)])
                        r = self.tmp()
                        self.rstd_from(pst[:], 128.0, r[:])
                        si = self.stage()
                        k.stt(self.stg[si][:], pss[t][:], vc(gname), r[:], ALU.mult, ALU.mult)
                        self.store(dst.v(dst.ap[h, :, tsl(t)], (h, t)), si)

        if STOP == 'mqk':
            return
        for blk in range(4):
            res = self.wst.next([self.wload(w_in, 0, D, MV0 + blk * 256, 256, 0)], [(0, NCH, 256, 128)])
            if k.planning:
                continue
            (wt,) = res
            for t in range(NT):
                for sb in range(4):
                    ps = k.ps()
                    k.mm_group(ps[:, 0:256], [(TV(self.HT[c][t].t[:, sb * 128:(sb + 1) * 128], [self.HT[c][t].buf]),
                                               TV(wt.ap[:, c, :], wt.bufs)) for c in range(NCH)])
                    si = self.stage()
                    k.copy(self.stg[si][:, 0:256], ps[:, 0:256])
                    r0 = t * TT + sb * 128
                    self.store(vcd.v(vcd.ap[r0:r0 + 128, blk * 256:(blk + 1) * 256], (r0, blk)), si, self.stg[si][:, 0:256])

        if STOP == 'mv':
            return
        for i in range(8):
            res = self.wst.next([self.wload(w_in, 0, D, CONV0 + i * 128, 128, 0),
                                 self.wload(w_in, 0, D, CONV0 + 1024 + i * 128, 128, 2048)],
                                [(0, NCH, 128, 128), (2048, NCH, 128, 128)])
            if k.planning:
                continue
            wa, wg_ = res
            psa = [k.ps() for _ in range(NT)]
            psg = [k.ps() for _ in range(NT)]
            self.mm2([p[:] for p in psa], wa, NCH, (0, 128), self.HT)
            self.mm2([p[:] for p in psg], wg_, NCH, (0, 128), self.HT)
            for t in range(NT):
                sg = self.tmp()
                k.act(sg[:], psg[t][:], AF.Sigmoid)
                zi = self.tmp_i()
                k.tt(self.tmpf[zi][:], sg[:], psa[t][:], ALU.mult)
                k.dma(k.SP, zc.v(zc.ap[i * 128:(i + 1) * 128, tsl(t)], (i, t)), self.tmpf[zi][:], self.tmpsem[zi])

        if STOP == 'conv':
            return
        for b in range(3):
            for cb in range(8):
                res = self.wst.next([self.wload(w_in, 0, D, G0 + b * D + cb * 256, 256, 0)], [(0, NCH, 256, 128)])
                if k.planning:
                    continue
                (wt,) = res
                for i in range(2):
                    c = cb * 2 + i
                    pss = [k.ps() for _ in range(NT)]
                    self.mm2([p[:] for p in pss], wt, NCH, (i * 128, i * 128 + 128), self.HT)
                    for t in range(NT):
                        si = self.stage()
                        k.act(self.stg[si][:], pss[t][:], AF.Sigmoid, bias=vc("b_gate", b * 16 + c))
                        self.store(gt.v(gt.ap[b, c * 128:(c + 1) * 128, tsl(t)], (b, c, t)), si)

    def attn_tiles(self, h, t, moba, Kn, Kr, Vv, Qn, Qr, penT, dst):
        k = self.k
        acc_o = k.psum[4 + 2 * (t % 2)]
        acc_d = k.psum[5 + 2 * (t % 2)]
        if self.fused and self.v == 0:
            if t == 0:
                blocks = [("l", j) for j in range(0, 4)]
            else:
                blocks = [("l", 0)] + [("l", j) for j in range(4, 8)] + [("l", j) for j in range(1, 4)]
        else:
            blocks = [("r", 0)] + [("l", j) for j in range(4 * t, 4 * t + 4)] + [("r", kb) for kb in range(1, 8)] + \
                     [("l", j) for j in range(0, 4 * t)]
        slope = 2.0 ** (-(h + 1))
        nblk = len(blocks)
        LA = 3
        pend = []

        def score(bi, kind, j):
            kcol = j * 128 if kind == "r" else T + j * 128
            vblk = j if kind == "r" else 8 + j
            r = (j - 4 * t) if (kind == "l" and j >= 4 * t) else None
            q0 = 128 * r if r is not None else 0
            qs = slice(t * TT + q0, (t + 1) * TT)
            ps = k.ps()
            pairs = [(TV(Kn.ap[:, 0, kcol:kcol + 128], Kn.bufs), TV(Qn.ap[:, 0, qs], Qn.bufs))]
            if not moba:
                pairs.append((TV(Kr.ap[0:64, 0, kcol:kcol + 128], Kr.bufs), TV(Qr.ap[0:64, 0, qs], Qr.bufs)))
            else:
                slot = (j // 2) if kind == "r" else 4 + j // 2
                pairs.append((self.cbf[0:8, 128 + slot * 128:128 + (slot + 1) * 128], penT[0:8, qs]))
            k.mm_group(ps[:, q0:TT], pairs)
            pi = self.stage()
            pt = self.stg[pi]
            if not moba:
                if r is None:
                    bias = self.cs[:, 0:1] if kind == "r" else None
                    k.act(pt[:, q0:TT], ps[:, q0:TT], AF.Exp, bias=bias, scale=MLA_SCALE)
                else:
                    tm = self.tmp()
                    w0 = (3 - r) * 128
                    k.tt(tm[:, q0:TT], ps[:, q0:TT], self.cbuf[:, w0 + q0:w0 + TT], ALU.add)
                    k.act(pt[:, q0:TT], tm[:, q0:TT], AF.Exp, scale=MLA_SCALE)
            else:
                tm = self.tmp()
                if r is None:
                    delta = ((T + t * TT) - j * 128) if kind == "r" else (t * TT - j * 128)
                    k.stt(tm[:, :], self.cbuf[:, 1792:2304], slope / MOBA_SCALE, ps[:, :], ALU.mult, ALU.add)
                    k.act(pt[:, :], tm[:, :], AF.Exp, bias=-slope * delta, scale=MOBA_SCALE)
                else:
                    w0 = 896 + (3 - r) * 128
                    k.stt(tm[:, q0:TT], self.cbuf[:, w0 + q0:w0 + TT], slope / MOBA_SCALE, ps[:, q0:TT], ALU.mult, ALU.add)
                    k.act(pt[:, q0:TT], tm[:, q0:TT], AF.Exp, scale=MOBA_SCALE)
            return (bi, vblk, pt, q0)

        def accum(bi, vblk, pt, q0):
            first, last = (bi == 0), (bi == nblk - 1)
            k.op(k.PE, (lambda: k.nc.tensor.matmul(acc_o.t[:, q0:TT], lhsT=Vv.ap[:, vblk, :], rhs=pt.t[:, q0:TT],
                                                   start=first, stop=last)), [Vv, pt[:]], [acc_o[:]])
            k.op(k.PE, (lambda: k.nc.tensor.matmul(acc_d.t[:, q0:TT], lhsT=self.ones.t[:], rhs=pt.t[:, q0:TT],
                                                   start=first, stop=last)), [self.ones[:], pt[:]], [acc_d[:]])

        for bi, (kind, j) in enumerate(blocks):
            pend.append(score(bi, kind, j))
            if len(pend) > LA:
                accum(*pend.pop(0))
        while pend:
            accum(*pend.pop(0))
        rec = self.tmp()
        k.recip(rec[:], acc_d[:])
        k.tt(dst[:], acc_o[:], rec[:], ALU.mult)

    def moba_select(self, Kn, Qn, penT):
        k = self.k
        km, kmb = self.km, self.kmb
        k.op(k.DVE, lambda: k.nc.vector.tensor_reduce(out=km.t[:, 0:8], in_=Kn.ap[:, 0, :].rearrange("p (s n) -> p s n", s=8),
                                                       axis=AX.X, op=ALU.add), [Kn], [km[:]])
        k.ts(kmb[:, 0:8], km[:, 0:8], 1.0 / 256.0, None, ALU.mult)
        ps = k.ps()

        def gates():
            ins = None
            for s_ in range(8):
                ins = k.nc.tensor.matmul(ps.t[:, s_ * 8:(s_ + 1) * 8], lhsT=Qn.ap[:, 0, s_ * 128:(s_ + 1) * 128],
                                         rhs=kmb.t[:, 0:8], start=True, stop=True)
            return ins
        k.op(k.PE, gates, [Qn, kmb[:]], [ps[:]])
        gm, cnt, cmp_, pen, penb = self.gm, self.cnt, self.cmp, self.pen, self.penb
        k.tt(gm[:], ps[:, 0:64], self.cs[:, 2:66], ALU.add)
        gm3 = gm.t[:].rearrange("p (s n) -> p s n", s=8)
        for m in range(8):
            bc = TV(gm3[:, :, m:m + 1].to_broadcast([128, 8, 8]), [gm.buf])
            dst = cnt if m == 0 else cmp_
            k.tt(TV(dst.t[:].rearrange("p (s n) -> p s n", s=8), [dst.buf]), TV(gm3, [gm.buf]), bc, ALU.is_lt)
            if m > 0:
                k.tt(cnt[:], cnt[:], cmp_[:], ALU.add)
        k.tt(cnt[:], cnt[:], self.cs[:, 130:194], ALU.mult)
        k.ts(pen[:], cnt[:], 3.0, NEG, ALU.is_ge, ALU.mult)
        k.tt(penb[:], pen[:], self.cs[:, 66:130], ALU.add)
        for hf in range(2):
            ps2 = k.ps()

            def tr(hf=hf, ps2=ps2):
                ins = None
                for s4 in range(4):
                    s_ = hf * 4 + s4
                    ins = k.nc.tensor.matmul(ps2.t[0:8, s4 * 128:(s4 + 1) * 128], lhsT=penb.t[:, s_ * 8:(s_ + 1) * 8],
                                             rhs=self.cbf.t[:, 0:128], start=True, stop=True)
                return ins
            k.op(k.PE, tr, [penb[:], self.cbf[:, 0:128]], [ps2[0:8, :]])
            k.copy(penT[0:8, hf * TT:(hf + 1) * TT], ps2[0:8, :])

    def conv_chunk(self, l, i, ZC):
        k = self.k
        VB = l * VL["_per_layer"]
        zc = self.S("zc", l, "r")
        zr = self.S("zc", l, "rr")
        zb = self.zb
        wcol = VB + VL["conv_w"] + i * 31
        bcol = VB + VL["conv_b"] + i
        for t in range(NT):
            if t == 0:
                k.dma(k.SP, zb[:, 30:30 + TT], zc.v(zc.ap[i * 128:(i + 1) * 128, 0:TT], (i, 0)), self.zbsem)
                k.dma(k.SP, zb[:, 0:30], zr.v(zr.ap[i * 128:(i + 1) * 128, T - 30:T], (i, NT - 1)), self.zbsem)
                k.ts(zb[:, 0:30], zb[:, 0:30], self.cs[:, 1:2], None, ALU.mult)
            else:
                k.dma(k.SP, zb[:, 0:30 + TT], zc.v(zc.ap[i * 128:(i + 1) * 128, t * TT - 30:(t + 1) * TT], (i, t - 1), (i, t)),
                      self.zbsem)
            accA, accB = self.tmp(), self.tmp()
            k.ts(accA[:], zb[:, 0:TT], self.vecs[:, wcol:wcol + 1], self.vecs[:, bcol:bcol + 1], ALU.mult, ALU.add)
            k.ts(accB[:], zb[:, 1:1 + TT], self.vecs[:, wcol + 1:wcol + 2], None, ALU.mult)
            for tap in range(2, 31):
                acc = accA if tap % 2 == 0 else accB
                k.stt(acc[:], zb[:, tap:tap + TT], self.vecs[:, wcol + tap:wcol + tap + 1], acc[:], ALU.mult, ALU.add)
            k.tt(ZC[i][t][:], accA[:], accB[:], ALU.add)

    def conv_finish(self, l, ZC):
        k = self.k
        VB = l * VL["_per_layer"]
        for t in range(NT):
            s1, s2 = k.psum[4], k.psum[5]
            for i in range(8):
                k.act(self.sq[i % 2][:], ZC[i][t][:], AF.Square)
                k.op(k.PE, (lambda i=i: k.nc.tensor.matmul(s1.t[:], lhsT=self.ones.t[:], rhs=ZC[i][t].t[:],
                                                            start=(i == 0), stop=(i == 7))), [self.ones[:], ZC[i][t][:]], [s1[:]])
                k.op(k.PE, (lambda i=i: k.nc.tensor.matmul(s2.t[:], lhsT=self.ones.t[:], rhs=self.sq[i % 2].t[:],
                                                            start=(i == 0), stop=(i == 7))), [self.ones[:], self.sq[i % 2][:]], [s2[:]])
            mean, var = self.rstd[0], self.rstd[1]
            k.ts(mean[:], s1[:], 1.0 / 1024.0, None, ALU.mult)
            msq = self.tmp()
            k.tt(msq[:], mean[:], mean[:], ALU.mult)
            k.stt(var[:], s2[:], 1.0 / 1024.0, msq[:], ALU.mult, ALU.subtract)
            self.rstd_from(var[:], 1.0, var[:])
            for i in range(8):
                tm = self.tmp()
                k.tt(tm[:], ZC[i][t][:], mean[:], ALU.subtract)
                k.tt(tm[:], tm[:], var[:], ALU.mult)
                k.act(ZC[i][t][:], tm[:], AF.Silu, bias=self.vecs[:, VB + VL["ln_b"] + i:VB + VL["ln_b"] + i + 1],
                      scale=self.vecs[:, VB + VL["ln_g"] + i:VB + VL["ln_g"] + i + 1])

    def mixer_out(self, l):
        k = self.k
        k.set_rr(4)
        self.load_cbuf("mask")
        OA = [[self.HT[h][t] for t in range(NT)] for h in range(8)]
        OC = [[self.HT[8 + h][t] for t in range(NT)] for h in range(8)]
        ZC = [[self.pool[i * NT + t] for t in range(NT)] for i in range(8)]
        qa, ka, va = self.S("qa", l, "r"), self.S("ka", l, "r"), self.S("va", l, "r")
        kar, var_ = self.S("ka", l, "rr"), self.S("va", l, "rr")
        qc, kc, vcd = self.S("qc", l, "r"), self.S("kc", l, "r"), self.S("vc", l, "r")
        kcr, vcr = self.S("kc", l, "rr"), self.S("vc", l, "rr")
        SPq = k.SP
        allk = lambda d, h, parts: [(h, t, p) for t in range(NT) for p in parts]
        def head_loads(u):
            moba = u >= 8
            h = u % 8
            if not moba:
                l1 = [LD(kar.v(kar.ap[h, 0:128, :].rearrange("(o p) n -> p o n", o=1), *allk(ka, h, (0,))), 0, 1, T, q=SPq),
                      LD(ka.v(ka.ap[h, 0:128, :].rearrange("(o p) n -> p o n", o=1), *allk(ka, h, (0,))), T, 1, T, q=SPq),
                      LD(kar.v(kar.ap[h, 128:192, :].rearrange("(o p) n -> p o n", o=1), *allk(ka, h, (1,))), 2 * T, 1, T, P=64, q=SPq),
                      LD(ka.v(ka.ap[h, 128:192, :].rearrange("(o p) n -> p o n", o=1), *allk(ka, h, (1,))), 3 * T, 1, T, P=64, q=SPq)]
                v1 = [(0, 1, 2 * T, 128), (2 * T, 1, 2 * T, 64)]
                vkeys = [(r0, g) for r0 in range(0, T, 128) for g in range(2)]
                l2 = [LD(var_.v(var_.ap[:, h * 128:(h + 1) * 128].rearrange("(b p) d -> p b d", p=128), *vkeys), 0, 8, 128, q=SPq),
                      LD(va.v(va.ap[:, h * 128:(h + 1) * 128].rearrange("(b p) d -> p b d", p=128), *vkeys), 1024, 8, 128, q=SPq),
                      LD(qa.v(qa.ap[h, 0:128, :].rearrange("(o p) n -> p o n", o=1), *allk(qa, h, (0,))), 2048, 1, T, q=SPq),
                      LD(qa.v(qa.ap[h, 128:192, :].rearrange("(o p) n -> p o n", o=1), *allk(qa, h, (1,))), 3072, 1, T, P=64, q=SPq)]
                v2 = [(0, 16, 128, 128), (2048, 1, T, 128), (3072, 1, T, 64)]
            else:
                hk = [(h, t) for t in range(NT)]
                l1 = [LD(kcr.v(kcr.ap[h, :, :].rearrange("(o p) n -> p o n", o=1), *hk), 0, 1, T, q=SPq),
                      LD(kc.v(kc.ap[h, :, :].rearrange("(o p) n -> p o n", o=1), *hk), T, 1, T, q=SPq)]
                v1 = [(0, 1, 2 * T, 128)]
                vkeys = [(r0, b) for r0 in range(0, T, 128) for b in range(4)]
                l2 = [LD(vcr.v(vcr.ap[:, h * 128:(h + 1) * 128].rearrange("(b p) d -> p b d", p=128), *vkeys), 0, 8, 128, q=SPq),
                      LD(vcd.v(vcd.ap[:, h * 128:(h + 1) * 128].rearrange("(b p) d -> p b d", p=128), *vkeys), 1024, 8, 128, q=SPq),
                      LD(qc.v(qc.ap[h, :, :].rearrange("(o p) n -> p o n", o=1), *hk), 2048, 1, T, q=SPq)]
                v2 = [(0, 16, 128, 128), (2048, 1, T, 128)]
            return l1, v1, l2, v2

        for h in range(8):
            l1, v1, l2, v2 = head_loads(h)
            r1 = self.wst.next(l1, v1)
            r2 = self.wst.next(l2, v2, cont=True)
            if not k.planning:
                Kn, Kr = r1
                Vv, Qn, Qr = r2
                for t in range(NT):
                    self.attn_tiles(h, t, False, Kn, Kr, Vv, Qn, Qr, None, OA[h][t])
            if h % 2 == 1:
                self.conv_chunk(l, h // 2, ZC)
        cur = None
        for h in range(8):
            if cur is None:
                i0 = self.wst.idx()
                l1, v1, l2, v2 = head_loads(8 + h)
                r1 = self.wst.next(l1, v1)
                r2 = self.wst.next(l2, v2, cont=True)
                cur = (i0, r1, r2, self.penT[h % 2])
                if not k.planning:
                    self.moba_select(r1[0], r2[1], self.penT[h % 2])
            i0, r1, r2, pT = cur
            nxt = None
            if h + 1 < 8:
                i1 = self.wst.idx()
                l1, v1, l2, v2 = head_loads(8 + h + 1)
                n1 = self.wst.next(l1, v1, anchor=i0)
                n2 = self.wst.next(l2, v2, anchor=i0)
                nxt = (i1, n1, n2, self.penT[(h + 1) % 2])
            if not k.planning:
                (Kn,) = r1
                Vv, Qn = r2
                self.attn_tiles(h, 0, True, Kn, None, Vv, Qn, None, pT, OC[h][0])
                if nxt is not None:
                    self.moba_select(nxt[1][0], nxt[2][1], nxt[3])
                self.attn_tiles(h, 1, True, Kn, None, Vv, Qn, None, pT, OC[h][1])
            if h % 2 == 1:
                self.conv_chunk(l, 4 + h // 2, ZC)
            cur = nxt
        self.conv_finish(l, ZC)

        k.set_rr(6)
        VB = l * VL["_per_layer"]
        wA, wB, wC, wO = self.W("mla_w_o", l), self.W("conv_w_pw", l), self.W("moba_w_o", l), self.W("w_out", l)
        gt = self.S("gt", l, "r")
        merged = [self.pool[16 + c] for c in range(16)]
        for t in range(NT):
            for c in range(NCH):
                ra = self.wst.next([self.wload(wA, 0, 1024, c * 128, 128, 0), self.wload(wB, 0, 1024, c * 128, 128, 1024),
                                    self.wload(wC, 0, 1024, c * 128, 128, 2048)],
                                   [(0, 8, 128, 128), (1024, 8, 128, 128), (2048, 8, 128, 128)])
                rb = self.wst.next([LD(gt.v(gt.ap[b, c * 128:(c + 1) * 128, t * TT:(t + 1) * TT].rearrange("(o p) n -> p o n", o=1),
                                            (b, c, t)), b * TT, 1, TT, q=SPq) for b in range(3)],
                                   [(b * TT, 1, TT, 128) for b in range(3)], cont=True)
                if k.planning:
                    continue
                srcs = (OA, ZC, OC)
                ys = []
                for b in range(3):
                    ps = k.ps()
                    k.mm_group(ps[:], [(TV(ra[b].ap[:, i, :], ra[b].bufs), srcs[b][i][t][:]) for i in range(8)])
                    ys.append(ps)
                m1, m2 = self.tmp(), self.tmp()
                k.tt(m1[:], ys[0][:], TV(rb[0].ap[:, 0, :], rb[0].bufs), ALU.mult)
                k.tt(m2[:], ys[1][:], TV(rb[1].ap[:, 0, :], rb[1].bufs), ALU.mult)
                k.tt(m1[:], m1[:], m2[:], ALU.add)
                k.tt(m2[:], ys[2][:], TV(rb[2].ap[:, 0, :], rb[2].bufs), ALU.mult)
                k.tt(merged[c][:], m1[:], m2[:], ALU.add)
            for d in range(NCH):
                res = self.wst.next([self.wload(wO, 0, D, d * 128, 128, 0)], [(0, NCH, 128, 128)])
                if k.planning:
                    continue
                (wo,) = res
                ps = k.ps()
                k.mm_group(ps[:], [(TV(wo.ap[:, c, :], wo.bufs), merged[c][:]) for c in range(NCH)])
                x = self.XT[d][t]
                k.tt(x[:], x[:], ps[:], ALU.add)

    def seg_A(self, l):
        self.ffn(l, "ffn1")
        self.mixer_in(l)

    def seg_B(self, l):
        self.mixer_out(l)
        self.ffn(l, "ffn2")


def build_program(segs, first, last, fused=False):
    nc = bass.Bass("TRN2", target_bir_lowering=False)
    es = ExitStack()
    with es:
        k = KB(nc, es)
        p = Prog(k, fused)

        def body():
            for (kind, l) in segs:
                if kind == "A":
                    p.seg_A(l)
                elif kind == "M1":
                    p.mixer_in(l)
                elif kind == "M2":
                    p.mixer_out(l)
                else:
                    p.seg_B(l)
        k.planning = True
        body()
        k.planning = False
        p.setup()
        p.load_x()
        body()
        p.store_x()
        p.finish()
        info = (list(p.in_names), list(p.out_names), dict(k.stats))
    return nc, info


def build_fused(depth=DEPTH):
    nc = bass.Bass("TRN2", target_bir_lowering=False)
    es = ExitStack()
    with es:
        k = KB(nc, es)
        p = Prog(k, True)

        def body():
            for l in range(depth):
                for v in range(2):
                    p.set_half(v)
                    p.load_xs(l == 0)
                    p.seg_A(l)
                    p.store_xs(False)
                for v in range(2):
                    p.set_half(v)
                    p.load_xs(False)
                    p.seg_B(l)
                    p.store_xs(l == depth - 1)
        k.planning = True
        body()
        k.planning = False
        p.setup()
        body()
        p.finish()
        info = (list(p.in_names), list(p.out_names), dict(k.stats))
    return nc, info


def pack_vecs(inputs):
    per = VL["_per_layer"]
    v = np.zeros((128, NVEC), np.float32)

    def put(l, name, arr):
        arr = np.asarray(arr, np.float32)
        v[:arr.shape[0], l * per + VL[name]: l * per + VL[name] + arr.shape[1]] = arr

    for l in range(DEPTH):
        for name in ("ffn1_norm", "mix_norm", "ffn2_norm"):
            put(l, name, np.asarray(inputs[name][l]).reshape(16, 128).T)
        put(l, "b_gate", np.asarray(inputs["b_gate"][l]).reshape(48, 128).T)
        put(l, "cq_norm", np.asarray(inputs["mla_cq_norm"][l]).reshape(6, 128).T)
        put(l, "ckv_norm", np.asarray(inputs["mla_ckv_norm"][l]).reshape(4, 128).T)
        for pre, src in (("qn", "mla_q_norm"), ("kn", "mla_k_norm")):
            g = np.asarray(inputs[src][l], np.float32)
            put(l, pre + "_nope", g[:128].reshape(128, 1))
            put(l, pre + "_rope", g[128:192].reshape(64, 1))
            put(l, pre + "_rperm", np.concatenate([g[160:192], g[128:160]]).reshape(64, 1))
        w = np.asarray(inputs["conv_w_dw"][l], np.float32)
        put(l, "conv_w", w.T.reshape(8, 128, 31).transpose(1, 0, 2).reshape(128, 248))
        put(l, "conv_b", np.asarray(inputs["conv_b_dw"][l]).reshape(8, 128).T)
        put(l, "ln_g", np.asarray(inputs["conv_ln_g"][l]).reshape(8, 128).T)
        put(l, "ln_b", np.asarray(inputs["conv_ln_b"][l]).reshape(8, 128).T)
        put(l, "mq_norm", np.asarray(inputs["moba_q_norm"][l]).reshape(128, 1))
        put(l, "mk_norm", np.asarray(inputs["moba_k_norm"][l]).reshape(128, 1))
    return v


def make_consts(half):
    import ml_dtypes
    pos = (half * T + np.arange(T)).astype(np.float32)
    inv = np.exp(-np.log(10000.0) * np.arange(32, dtype=np.float32) * 2.0 / 64.0).astype(np.float32)
    ang = (pos[None, :] * inv[:, None]).astype(np.float32)
    cos, sin = np.cos(ang), np.sin(ang)
    rope = np.zeros((64, 2 * T), np.float32)
    rope[0:32, 0:T] = cos
    rope[32:64, 0:T] = cos
    rope[0:32, T:] = -sin
    rope[32:64, T:] = sin
    key = np.arange(128, dtype=np.float32)[:, None]
    j = np.arange(896, dtype=np.float32)[None, :] - 384.0
    mask = np.zeros((128, CB_COLS), np.float32)
    mask[:, 0:896] = np.where(key <= j, 0.0, NEG)
    mask[:, 896:1792] = np.where(key <= j, key - j, -1.0e6)
    mask[:, 1792:2304] = key - np.arange(512, dtype=np.float32)[None, :]
    cs = np.zeros((128, CS_COLS), np.float32)
    cs[:, 0] = 0.0 if half == 1 else NEG
    cs[:, 1] = 1.0 if half == 1 else 0.0
    for s in range(8):
        qbl = s // 2
        for slot in range(8):
            if slot < 4:
                past, own = (half == 1), False
            else:
                past, own = (slot - 4 < qbl), (slot - 4 == qbl)
            cs[:, 2 + s * 8 + slot] = 0.0 if past else -1.0e30
            cs[:, 66 + s * 8 + slot] = 0.0 if (past or own) else NEG
            cs[:, 130 + s * 8 + slot] = 0.0 if own else 1.0
    cbf = np.zeros((128, CBF_COLS), np.float32)
    cbf[:, 0:128] = np.eye(128, dtype=np.float32)
    for slot in range(8):
        cbf[slot, 128 + slot * 128:128 + (slot + 1) * 128] = 1.0
    return {"rope_in": rope, "mask_in": mask, "cs_in": cs, "cbf_in": cbf.astype(ml_dtypes.bfloat16)}


N_CORES = 4


def kernel(**inputs):
    x = np.asarray(inputs["x"], np.float32)
    vecs = pack_vecs(inputs)
    c0, c1 = make_consts(0), make_consts(1)
    nc, (in_names, out_names, stats) = build_fused()
    shared = {"vecs_in": vecs, "cbf_in": c0["cbf_in"], "mask_in": c0["mask_in"],
              "rope_in": np.stack([c0["rope_in"], c1["rope_in"]]), "cs_in": np.stack([c0["cs_in"], c1["cs_in"]])}
    for name in in_names:
        if name not in shared and name != "xT":
            base, l = name.rsplit("_", 1)
            shared[name] = np.ascontiguousarray(np.asarray(inputs[base][int(l)], np.float32))
    in_maps = []
    for b in range(N_CORES):
        m = {name: shared[name] for name in in_names if name != "xT"}
        m["xT"] = np.ascontiguousarray(x[b].reshape(2, T, D).transpose(0, 2, 1))
        in_maps.append(m)
    res = run_bass_kernel_spmd(nc, in_maps, core_ids=list(range(N_CORES)))
    out = np.empty((NB, SEQ, D), np.float32)
    for b in range(N_CORES):
        out[b] = res.results[b]["yT"].transpose(0, 2, 1).reshape(SEQ, D)
    return out
```
